# Optimizing a Trainium2 kernel written in Bass

```python
import jax, jax.numpy as jnp
from jax import lax
import numpy as np

D_MODEL = 2048
BATCH = 4
SEQ = 2048
DEPTH = 1

D_MIX = D_MODEL
CHUNK = 128
SGU_GROUPS = 8
SGU_DIM = (D_MIX // 2) // SGU_GROUPS
SGU_WIDTH = SGU_GROUPS * SGU_DIM
HEAD_DIM = 64
N_Q_HEADS = (D_MIX // 2) // HEAD_DIM
N_KV_HEADS = 2
WINDOW = 128
ATTN_WIDTH = N_Q_HEADS * HEAD_DIM
KV_WIDTH = N_KV_HEADS * HEAD_DIM
IN_WIDTH = 2 * SGU_WIDTH + ATTN_WIDTH + 2 * KV_WIDTH
PEER_HEADS = 8
N_KEYS = 128
N_EXPERTS = N_KEYS * N_KEYS
PEER_TOPK = 16
D_QUERY = 256
EXPERT_BLOCK = 128
EPS = 1e-6

kernel_name = "hybrid_gmlp_swa_sink_peer_block"


def rmsnorm(x, g):
    xf = x.astype(jnp.float32)
    y = xf * lax.rsqrt(jnp.mean(xf * xf, axis=-1, keepdims=True) + EPS)
    return (y * g.astype(jnp.float32)).astype(x.dtype)


def chunked_spatial_gating(u, v, ln_g, ln_b, w_s, b_s):
    B, S, _ = u.shape
    nc = S // CHUNK
    u = u.reshape(B, nc, CHUNK, SGU_GROUPS, SGU_DIM)
    v = v.reshape(B, nc, CHUNK, SGU_GROUPS, SGU_DIM)
    vf = v.astype(jnp.float32)
    mu = jnp.mean(vf, axis=-1, keepdims=True)
    var = jnp.mean(jnp.square(vf - mu), axis=-1, keepdims=True)
    vn = ((vf - mu) * lax.rsqrt(var + EPS) * ln_g.astype(jnp.float32) + ln_b.astype(jnp.float32)).astype(u.dtype)
    causal = jnp.tril(jnp.ones((CHUNK, CHUNK), dtype=bool))
    w = jnp.where(causal[None], w_s, jnp.zeros_like(w_s))
    s = jnp.einsum('gts,bcsgd->bctgd', w, vn) + b_s.T[None, None, :, :, None]
    return (u * s).reshape(B, S, SGU_WIDTH)


def sliding_window_attention(q, k, v, sinks):
    B, S = q.shape[0], q.shape[1]
    nb = S // WINDOW
    G = N_Q_HEADS // N_KV_HEADS
    qb = q.reshape(B, nb, WINDOW, N_KV_HEADS, G, HEAD_DIM)
    kb = k.reshape(B, nb, WINDOW, N_KV_HEADS, HEAD_DIM)
    vb = v.reshape(B, nb, WINDOW, N_KV_HEADS, HEAD_DIM)
    pad = ((0, 0), (1, 0), (0, 0), (0, 0), (0, 0))
    kk = jnp.concatenate([jnp.pad(kb[:, :-1], pad), kb], axis=2)
    vv = jnp.concatenate([jnp.pad(vb[:, :-1], pad), vb], axis=2)
    scores = jnp.einsum('bnqhgd,bnkhd->bnhgqk', qb, kk).astype(jnp.float32) * (HEAD_DIM ** -0.5)
    i = jnp.arange(WINDOW)[:, None]
    j = jnp.arange(2 * WINDOW)[None, :]
    band = (j >= i + 1) & (j <= i + WINDOW)
    blk = jnp.arange(nb)[:, None, None]
    mask = band[None] & ((blk > 0) | (j >= WINDOW)[None])
    scores = jnp.where(mask[None, :, None, None], scores, -jnp.inf)
    sink = sinks.astype(jnp.float32).reshape(N_KV_HEADS, G)[None, None, :, :, None, None]
    m = jnp.maximum(jnp.max(scores, axis=-1, keepdims=True), sink)
    p = jnp.exp(scores - m)
    denom = jnp.sum(p, axis=-1, keepdims=True) + jnp.exp(sink - m)
    probs = (p / denom).astype(v.dtype)
    out = jnp.einsum('bnhgqk,bnkhd->bnqhgd', probs, vv)
    return out.reshape(B, S, ATTN_WIDTH)


def peer(h, w_query, sub_keys, expert_down, expert_up):
    B, S, D = h.shape
    T = B * S
    xf = h.reshape(T, D)
    q = (xf @ w_query).reshape(T, PEER_HEADS, 2, D_QUERY // 2)
    s1 = jnp.einsum('thd,kd->thk', q[:, :, 0], sub_keys[0]).astype(jnp.float32)
    s2 = jnp.einsum('thd,kd->thk', q[:, :, 1], sub_keys[1]).astype(jnp.float32)
    v1, i1 = lax.top_k(s1, PEER_TOPK)
    v2, i2 = lax.top_k(s2, PEER_TOPK)
    cand = (v1[..., :, None] + v2[..., None, :]).reshape(T, PEER_HEADS, PEER_TOPK * PEER_TOPK)
    top_v, flat = lax.top_k(cand, PEER_TOPK)
    e1 = jnp.take_along_axis(i1, flat // PEER_TOPK, axis=-1)
    e2 = jnp.take_along_axis(i2, flat % PEER_TOPK, axis=-1)
    experts = e1 * N_KEYS + e2
    gates = jax.nn.softmax(top_v, axis=-1).astype(h.dtype)
    nblk = T // EXPERT_BLOCK

    def block(args):
        xc, ec, gc = args
        u = jnp.take(expert_down, ec, axis=0)
        a = jax.nn.gelu(jnp.einsum('cd,chkd->chk', xc, u), approximate=False)
        vt = jnp.take(expert_up, ec, axis=0)
        return jnp.einsum('chk,chkd->cd', gc * a, vt)

    out = lax.map(block, (xf.reshape(nblk, EXPERT_BLOCK, D),
                          experts.reshape(nblk, EXPERT_BLOCK, PEER_HEADS, PEER_TOPK),
                          gates.reshape(nblk, EXPERT_BLOCK, PEER_HEADS, PEER_TOPK)))
    return out.reshape(B, S, D)


def setup_inputs(seed: int = 0) -> dict:
    key = jax.random.key(seed)
    ks = jax.random.split(key, 16)
    L = DEPTH

    def nrm(k, shape, scale):
        return jax.random.normal(k, shape, jnp.float32) * scale

    return {
        "x": nrm(ks[0], (BATCH, SEQ, D_MODEL), 1.0),
        "norm1_g": 1.0 + nrm(ks[1], (L, D_MODEL), 0.01),
        "w_in": nrm(ks[2], (L, D_MODEL, IN_WIDTH), D_MODEL ** -0.5),
        "sgu_ln_g": 1.0 + nrm(ks[3], (L, SGU_GROUPS, SGU_DIM), 0.01),
        "sgu_ln_b": nrm(ks[4], (L, SGU_GROUPS, SGU_DIM), 0.01),
        "w_spatial": nrm(ks[5], (L, SGU_GROUPS, CHUNK, CHUNK), CHUNK ** -0.5),
        "b_spatial": 1.0 + nrm(ks[6], (L, SGU_GROUPS, CHUNK), 0.01),
        "attn_sinks": nrm(ks[7], (L, N_Q_HEADS), 0.5),
        "w_out": nrm(ks[8], (L, D_MIX, D_MODEL), D_MIX ** -0.5),
        "norm2_g": 1.0 + nrm(ks[9], (L, D_MODEL), 0.01),
        "w_query": nrm(ks[10], (L, D_MODEL, PEER_HEADS * D_QUERY), D_MODEL ** -0.5),
        "sub_keys": nrm(ks[11], (L, 2, N_KEYS, D_QUERY // 2), (D_QUERY // 2) ** -0.5),
        "expert_down": nrm(ks[12], (L, N_EXPERTS, D_MODEL), D_MODEL ** -0.5),
        "expert_up": nrm(ks[13], (L, N_EXPERTS, D_MODEL), PEER_HEADS ** -0.5),
        "norm_f_g": 1.0 + nrm(ks[14], (D_MODEL,), 0.01),
    }


def reference(x, norm1_g, w_in, sgu_ln_g, sgu_ln_b, w_spatial, b_spatial, attn_sinks,
              w_out, norm2_g, w_query, sub_keys, expert_down, expert_up, norm_f_g):
    B, S, _ = x.shape
    splits = [SGU_WIDTH, 2 * SGU_WIDTH, 2 * SGU_WIDTH + ATTN_WIDTH,
              2 * SGU_WIDTH + ATTN_WIDTH + KV_WIDTH]
    for l in range(DEPTH):
        h = rmsnorm(x, norm1_g[l])
        z = h @ w_in[l]
        zu, zv, zq, zk, zvv = jnp.split(z, splits, axis=-1)
        a_out = chunked_spatial_gating(jax.nn.gelu(zu, approximate=False),
                                       jax.nn.gelu(zv, approximate=False),
                                       sgu_ln_g[l], sgu_ln_b[l], w_spatial[l], b_spatial[l])
        b_out = sliding_window_attention(zq.reshape(B, S, N_Q_HEADS, HEAD_DIM),
                                         zk.reshape(B, S, N_KV_HEADS, HEAD_DIM),
                                         zvv.reshape(B, S, N_KV_HEADS, HEAD_DIM),
                                         attn_sinks[l])
        x = x + jnp.concatenate([a_out, b_out], axis=-1) @ w_out[l]
        h2 = rmsnorm(x, norm2_g[l])
        x = x + peer(h2, w_query[l], sub_keys[l], expert_down[l], expert_up[l])
    return rmsnorm(x, norm_f_g)
```

```python
import contextlib
import numpy as np
import concourse.bass as bass
import concourse.mybir as mybir
from concourse.bass_utils import run_bass_kernel_spmd

F32 = mybir.dt.float32
BF16 = mybir.dt.bfloat16
U32 = mybir.dt.uint32
AF = mybir.ActivationFunctionType
ALU = mybir.AluOpType
AX = mybir.AxisListType

NCORES = 8
T = 1024
TH = 1152
D = 2048
KC = 16
EPS = 1e-6
NEG = -30000.0


_NEED = None


class Sched:
    ENGS = ("pe", "act", "dve", "pool", "sp")
    last_need = None

    def __init__(self, nc):
        import bisect
        self._bisect = bisect
        self.nc = nc
        self.dry = _NEED is None
        self.need = {e: set() for e in ("pe", "act", "dve", "pool")} if self.dry else None
        self.rank = None if self.dry else {e: sorted(v) for e, v in _NEED.items()}
        self.needset = None if self.dry else _NEED
        self.streams = {e: [] for e in self.ENGS}
        self.cnt = {}
        self.semh = {}
        self.waited = {e: {} for e in self.ENGS}
        self.last_w = {}
        self.readers = {}
        self._ctx = []
        for e in ("pe", "act", "dve", "pool"):
            self._mk_sem(e)

    def _mk_sem(self, name):
        cm = self.nc.semaphore("s_" + name)
        h = cm.__enter__()
        self._ctx.append(cm)
        self.semh[name] = h
        self.cnt[name] = 0

    def _val(self, dom, c):
        if dom.startswith("d_"):
            return c
        if self.dry:
            self.need[dom].add(c)
            return c
        return self._bisect.bisect_right(self.rank[dom], c)

    def _deps(self, eng, reads, writes):
        deps = {}

        def add(dom, c):
            if dom.startswith("d_"):
                c = self.cnt[dom]
            deps[dom] = max(deps.get(dom, 0), c)
        for k in reads:
            lw = self.last_w.get(k)
            if lw:
                add(*lw)
        for k in writes:
            lw = self.last_w.get(k)
            if lw and lw[0] != eng:
                add(*lw)
            for dom, c in self.readers.get(k, {}).items():
                if dom != eng:
                    add(dom, c)
        out = []
        for dom, c in deps.items():
            if self.waited[eng].get(dom, 0) < c:
                self.waited[eng][dom] = c
                out.append((dom, self._val(dom, c)))
        return out

    def _record(self, dom, my, reads, writes):
        for k in reads:
            self.readers.setdefault(k, {})[dom] = my
        for k in writes:
            self.last_w[k] = (dom, my)
            self.readers[k] = {}

    def op(self, eng, fn, reads=(), writes=()):
        waits = self._deps(eng, reads, writes)
        self.cnt[eng] += 1
        my = self.cnt[eng]
        sem = self.semh[eng]
        semh = self.semh
        sig = self.dry or (my in self.needset[eng])

        def thunk(E):
            for dom, v in waits:
                E.wait_ge(semh[dom], v)
            ins = fn(E)
            if sig:
                ins.then_inc(sem, 1)
        self.streams[eng].append(thunk)
        self._record(eng, my, reads, writes)

    def dma(self, q, dsem, fn, reads=(), writes=()):
        dom = "d_" + dsem
        if dom not in self.semh:
            self._mk_sem(dom)
        waits = self._deps(q, reads, writes)
        self.cnt[dom] += 16
        my = self.cnt[dom]
        sem = self.semh[dom]
        semh = self.semh

        def thunk(E):
            for d, v in waits:
                E.wait_ge(semh[d], v)
            fn(E).then_inc(sem, 16)
        self.streams[q].append(thunk)
        self._record(dom, my, reads, writes)

    def barrier(self):
        snap = dict(self.cnt)
        semh = self.semh
        for e in self.ENGS:
            waits = []
            for dom, c in snap.items():
                if c > 0 and self.waited[e].get(dom, 0) < c:
                    self.waited[e][dom] = c
                    waits.append((dom, self._val(dom, c)))

            def thunk(E, waits=waits):
                for d, v in waits:
                    E.wait_ge(semh[d], v)
            self.streams[e].append(thunk)

    def finish(self):
        self.barrier()
        if self.dry:
            Sched.last_need = self.need
            for cm in reversed(self._ctx):
                cm.__exit__(None, None, None)
            return
        nc = self.nc
        streams = self.streams
        with nc.Block() as block:
            @block.tensor
            def _(E):
                for t in streams["pe"]:
                    t(E)

            @block.scalar
            def _(E):
                for t in streams["act"]:
                    t(E)

            @block.vector
            def _(E):
                for t in streams["dve"]:
                    t(E)

            @block.gpsimd
            def _(E):
                for t in streams["pool"]:
                    t(E)

            @block.sync
            def _(E):
                for t in streams["sp"]:
                    t(E)
        for cm in reversed(self._ctx):
            cm.__exit__(None, None, None)


class Arena:
    def __init__(self, nc, es, words):
        self.t = es.enter_context(nc.sbuf_tensor("arena", [128, words], F32))
        self.top = 0
        self.words = words

    def f32(self, n):
        n8 = (n + 7) // 8 * 8
        off = self.top
        self.top += n8
        assert self.top <= self.words, ("arena overflow", self.top, self.words)
        return self.t[:, off:off + n]

    def bf16(self, n):
        w = (n + 1) // 2
        w8 = (w + 7) // 8 * 8
        off = self.top
        self.top += w8
        assert self.top <= self.words, ("arena overflow", self.top, self.words)
        return self.t[:, off:off + w].bitcast(BF16)


def build(stage=99):
    global _NEED
    _NEED = None
    _build(stage)
    _NEED = Sched.last_need
    nc = _build(stage)
    _NEED = None
    return nc


def _build(stage=99):
    nc = bass.Bass("TRN2", target_bir_lowering=False)
    es = contextlib.ExitStack()

    def din(name, shape, dt=F32):
        return nc.dram_tensor(name, list(shape), dt, kind="ExternalInput").ap()

    xT_d = din("xT", [128, KC, TH])
    amask_d = din("amask", [128, 2, 256])
    w_u_d = din("w_u", [8, 128, KC, 128])
    w_v_d = din("w_v", [128, KC, 1024])
    w_q_d = din("w_q", [8, 128, KC, 128])
    w_k_d = din("w_k", [128, KC, 128])
    w_vv_d = din("w_vv", [128, KC, 128])
    gvec_d = din("gvec", [128, 3, KC])
    wsT_d = din("wsT", [128, 8, 128])
    bs_bc_d = din("bs_bc", [128, 8, 128])
    sink_bc_d = din("sink_bc", [128, 16])
    ln_d = din("ln_gb", [128, 2, 8])
    w_o_d = din("w_o", [16, 128, KC, 128])
    w_qr_d = din("w_qr", [16, 128, KC, 128])
    keysT_d = din("keysT", [128, 2, 128])
    if stage >= 20:
        dwn_d = din("dwn", [128, 128, KC, 128])
        if stage not in (21, 22):
            upw_d = din("upw", [2, 128, 128, 1024])
    cst_d = din("cst", [128, 3, 128])
    yT_d = nc.dram_tensor("yT", [128, KC, T], F32, kind="ExternalOutput").ap()
    x1s_d = nc.dram_tensor("x1s", [128, KC, T], F32, kind="ExternalOutput").ap()
    h2s_d = nc.dram_tensor("h2s", [128, KC, 512], BF16, kind="ExternalOutput").ap()

    S = Sched(nc)
    A = Arena(nc, es, 53200)
    psA = es.enter_context(nc.psum_tensor("psA", [128, 4096], F32))
    pb = [psA[:, i * 512:(i + 1) * 512] for i in range(8)]
    PB = [f"pb{i}" for i in range(8)]

    def mm(out, lhsT, rhs, start, stop, reads, writes):
        S.op("pe", lambda E: E.matmul(out, lhsT=lhsT, rhs=rhs, start=start, stop=stop), reads, writes)

    def tr(out, in_, ident, reads, writes):
        S.op("pe", lambda E: E.transpose(out, in_, ident), reads, writes)

    def act(out, in_, func, reads, writes, bias=None, scale=None, accum_out=None, eng="act"):
        kw = {}
        if bias is not None:
            kw["bias"] = bias
        if scale is not None:
            kw["scale"] = scale
        if accum_out is not None:
            kw["accum_out"] = accum_out
        S.op("act", lambda E: E.activation(out=out, in_=in_, func=func, **kw), reads, writes)

    def tt(eng, out, in0, in1, op, reads, writes):
        S.op(eng, lambda E: E.tensor_tensor(out=out, in0=in0, in1=in1, op=op), reads, writes)

    def ts(eng, out, in0, s1, op0, reads, writes, s2=None, op1=None):
        if op1 is None:
            S.op(eng, lambda E: E.tensor_scalar(out=out, in0=in0, scalar1=s1, scalar2=None, op0=op0), reads, writes)
        else:
            S.op(eng, lambda E: E.tensor_scalar(out=out, in0=in0, scalar1=s1, scalar2=s2, op0=op0, op1=op1), reads, writes)

    def stt(eng, out, in0, scalar, in1, op0, op1, reads, writes):
        S.op(eng, lambda E: E.scalar_tensor_tensor(out=out, in0=in0, scalar=scalar, in1=in1, op0=op0, op1=op1), reads, writes)

    def cp(eng, out, in_, reads, writes):
        if eng == "act":
            S.op("act", lambda E: E.activation(out=out, in_=in_, func=AF.Copy), reads, writes)
        else:
            S.op(eng, lambda E: E.tensor_copy(out, in_), reads, writes)

    def red(eng, out, in_, op, reads, writes):
        S.op(eng, lambda E: E.tensor_reduce(out=out, in_=in_, axis=AX.X, op=op), reads, writes)

    def dma(q, sem, out, in_, reads, writes):
        S.dma(q, sem, lambda E: E.dma_start(out=out, in_=in_), reads, writes)


    def dbg_exit(src_fn, nchunks, keys):
        stg = [A.f32(1024), A.f32(1024)]
        for a in range(nchunks):
            cp("dve", stg[a % 2], src_fn(a), keys, [f"dbg{a % 2}"])
            dma("sp", "y", yT_d[:, a, :], stg[a % 2], [f"dbg{a % 2}"], ["y"])
        S.finish()
        es.close()
        return nc

    cst = A.f32(3 * 128).rearrange("p (a b) -> p a b", b=128)
    ident_f = cst[:, 0, :]
    iota_f = cst[:, 1, :]
    cmask = cst[:, 2, :]
    ident_b = A.bf16(128)
    ones_b = A.bf16(128)
    gvec = A.f32(3 * KC).rearrange("p (a b) -> p a b", b=KC)
    keysT = A.bf16(2 * 128).rearrange("p (a b) -> p a b", b=128)
    dma("sp", "c0", cst, cst_d, [], ["cst"])
    dma("sp", "c0", gvec, gvec_d, [], ["gvec"])
    dma("pool", "c1", keysT, keysT_d, [], ["keysT"])
    cp("dve", ident_b, ident_f, ["cst"], ["ident_b"])
    S.op("dve", lambda E: E.memset(ones_b, 1.0), [], ["ones_b"])
    mark0 = A.top

    hT = A.bf16(KC * TH).rearrange("p (a b) -> p a b", b=TH)
    catT = A.bf16(16 * T).rearrange("p (a b) -> p a b", b=T)
    amask = A.f32(512).rearrange("p (a b) -> p a b", b=256)
    sink_bc = A.f32(16)
    ln_gb = A.f32(16).rearrange("p (a b) -> p a b", b=8)
    wsb = A.bf16(1024).rearrange("p (a b) -> p a b", b=128)
    Cg = A.f32(1024).rearrange("p (a b) -> p a b", b=128)
    rstd = A.f32(TH)
    NWB = 3
    wbuf = [A.bf16(KC * 128).rearrange("p (a b) -> p a b", b=128) for _ in range(NWB)]
    xstg = [A.f32(512) for _ in range(2)]
    markX = A.top
    XW = KC * TH
    assert markX + XW <= A.words

    wsrcs = ([w_u_d[g] for g in range(8)] + [w_q_d[c] for c in range(8)] + [w_k_d, w_vv_d]
             + [w_o_d[dc] for dc in range(16)])
    wstate = {"issued": 0, "used": 0}

    def get_w(limit=None):
        lim = len(wsrcs) if limit is None else limit
        while wstate["issued"] < min(lim, wstate["used"] + NWB):
            n = wstate["issued"]
            i = n % NWB
            dma("pool", f"wb{i}", wbuf[i], wsrcs[n], [], [f"wbuf{i}"])
            wstate["issued"] += 1
        i = wstate["used"] % NWB
        wstate["used"] += 1
        return wbuf[i], f"wbuf{i}"

    rot = {}

    def nextpb(lo=5, hi=8):
        i = rot.get((lo, hi), lo)
        rot[(lo, hi)] = lo + (i + 1 - lo) % (hi - lo)
        return i

    dma("sp", "c0", amask, amask_d, [], ["amask"])
    dma("sp", "c0", sink_bc, sink_bc_d, [], ["sink_bc"])
    dma("sp", "c0", ln_gb, ln_d, [], ["ln_gb"])

    A.top = markX
    xT = A.f32(KC * TH).rearrange("p (a b) -> p a b", b=TH)
    for i in range(4):
        dma("sp" if i % 2 == 0 else "act", "x", xT[:, 4 * i:4 * i + 4, :], xT_d[:, 4 * i:4 * i + 4, :], [], [f"xT{k}" for k in range(4 * i, 4 * i + 4)])
    w_vh_hi = A.t[:, markX + XW:markX + XW + 4096].bitcast(BF16).rearrange("p (a b) -> p a b", b=512)
    for q4 in range(4):
        dma("pool", "wv", w_vh_hi[:, 4 * q4:4 * q4 + 4, :], w_v_d[:, 4 * q4:4 * q4 + 4, 0:512], [], ["w_vh0"])
    CB = [(0, 512), (512, 512), (1024, 128)]
    for kc in range(KC):
        act(hT[:, kc, :], xT[:, kc, :], AF.Square, [f"xT{kc}"], [f"hT{kc}"])
        for bi, (c0, cn) in enumerate(CB):
            mm(pb[bi][:, 0:cn], ones_b, hT[:, kc, c0:c0 + cn], kc == 0, kc == KC - 1, ["ones_b", f"hT{kc}"], [PB[bi]])
    for bi, (c0, cn) in enumerate(CB):
        ts("dve", rstd[:, c0:c0 + cn], pb[bi][:, 0:cn], 1.0 / D, ALU.mult, [PB[bi]], ["rstd"], s2=EPS, op1=ALU.add)
    act(rstd, rstd, AF.Sqrt, ["rstd"], ["rstd"])
    S.op("dve", lambda E: E.reciprocal(out=rstd, in_=rstd), ["rstd"], ["rstd"])
    for kc in range(KC):
        stt("dve", hT[:, kc, :], xT[:, kc, :], gvec[:, 0, kc:kc + 1], rstd, ALU.mult, ALU.mult,
            [f"xT{kc}", "gvec", "rstd"], [f"hT{kc}"])
    S.barrier()
    if stage == 11:
        A.top = markX + XW
        return dbg_exit(lambda a: hT[:, a, 128:TH], 16, [f"hT{k}" for k in range(KC)])

    A.top = markX
    uT = A.bf16(8 * T).rearrange("p (a b) -> p a b", b=T)
    vn_all = A.bf16(8 * 1024).rearrange("p (a g c) -> p a g c", g=8, c=128)
    w_vh = A.bf16(KC * 512).rearrange("p (a b) -> p a b", b=512)
    vg = [A.f32(512).rearrange("p (a b) -> p a b", b=128) for _ in range(2)]
    cen = [A.f32(512).rearrange("p (a b) -> p a b", b=128) for _ in range(2)]
    sqv = A.f32(512).rearrange("p (a b) -> p a b", b=128)
    st8 = [A.f32(4 * 4).rearrange("p (a b) -> p a b", b=4) for _ in range(2)]
    sgt = [A.f32(512).rearrange("p (a b) -> p a b", b=128) for _ in range(2)]
    wsf = A.f32(1024).rearrange("p (a b) -> p a b", b=128)
    bs_bc = A.f32(1024).rearrange("p (a b) -> p a b", b=128)
    assert A.top <= markX + XW, A.top - markX
    dma("sp", "c0", wsf, wsT_d, [], ["wsf"])
    dma("sp", "c0", bs_bc, bs_bc_d, [], ["bs_bc"])
    tt("dve", wsb, wsf, cmask[:, None, :].broadcast_to([128, 8, 128]), ALU.mult, ["wsf", "cst"], ["wsb"])
    for hb in range(2):
        mm(pb[3 + hb][:, :], ones_b, wsb[:, 4 * hb:4 * hb + 4, :].rearrange("p a b -> p (a b)"), True, True,
           ["ones_b", "wsb"], [PB[3 + hb]])
    for g in range(8):
        stt("dve", Cg[:, g, :], pb[3 + g // 4][:, (g % 4) * 128:(g % 4 + 1) * 128], ln_gb[:, 1, g:g + 1], bs_bc[:, g, :],
            ALU.mult, ALU.add, [PB[3 + g // 4], "ln_gb", "bs_bc"], ["Cg"])
    for q4 in range(4):
        dma("pool", "wv", w_vh[:, 4 * q4:4 * q4 + 4, :], w_v_d[:, 4 * q4:4 * q4 + 4, 512:1024], [], ["w_vh1"])
    for hb in range(2):
        w_vb = w_vh_hi if hb == 0 else w_vh
        for tl in range(8):
            b = nextpb()
            i2 = tl % 2
            for kc in range(KC):
                mm(pb[b][:, :], hT[:, kc, 128 + tl * 128:128 + (tl + 1) * 128], w_vb[:, kc, :],
                   kc == 0, kc == KC - 1, [f"w_vh{hb}", f"hT{kc}"], [PB[b]])
            act(vg[i2].rearrange("p a b -> p (a b)"), pb[b][:, :], AF.Gelu, [PB[b]], [f"vg{i2}"])
            s8 = st8[i2]
            k8 = f"st8{i2}"
            red("dve", s8[:, 0, :], vg[i2], ALU.add, [f"vg{i2}"], [k8])
            ts("dve", s8[:, 1, :], s8[:, 0, :], -1.0 / 128, ALU.mult, [k8], [k8])
            tt("dve", cen[i2], vg[i2], s8[:, 1, :, None].broadcast_to([128, 4, 128]), ALU.add, [f"vg{i2}", k8], [f"cen{i2}"])
            tt("dve", sqv, cen[i2], cen[i2], ALU.mult, [f"cen{i2}"], ["sqv"])
            red("dve", s8[:, 2, :], sqv, ALU.add, ["sqv"], [k8])
            ts("dve", s8[:, 2, :], s8[:, 2, :], 1.0 / 128, ALU.mult, [k8], [k8], s2=EPS, op1=ALU.add)
            act(s8[:, 3, :], s8[:, 2, :], AF.Sqrt, [k8], [k8])
            S.op("dve", lambda E, s8=s8: E.reciprocal(out=s8[:, 3, :], in_=s8[:, 3, :]), [k8], [k8])
            tt("dve", vn_all[:, tl, 4 * hb:4 * hb + 4, :], cen[i2], s8[:, 3, :, None].broadcast_to([128, 4, 128]), ALU.mult,
               [f"cen{i2}", k8], [f"vn{tl}"])
    for g in range(8):
        wb, wk = get_w()
        for th in range(2):
            b = nextpb()
            for kc in range(KC):
                mm(pb[b][:, :], wb[:, kc, :], hT[:, kc, 128 + th * 512:128 + (th + 1) * 512], kc == 0, kc == KC - 1,
                   [wk, f"hT{kc}"], [PB[b]])
            act(uT[:, g, th * 512:(th + 1) * 512], pb[b][:, :], AF.Gelu, [PB[b]], ["uT"])
    for tl in range(8):
        for hb in range(2):
            b = nextpb()
            for g4 in range(4):
                g = hb * 4 + g4
                mm(pb[b][:, g4 * 128:(g4 + 1) * 128], vn_all[:, tl, g, :], wsb[:, g, :], True, True,
                   [f"vn{tl}", "wsb"], [PB[b]])
            sk = f"sgt{hb}"
            for g4 in range(4):
                g = hb * 4 + g4
                stt("dve", sgt[hb][:, g4, :], pb[b][:, g4 * 128:(g4 + 1) * 128], ln_gb[:, 0, g:g + 1], Cg[:, g, :],
                    ALU.mult, ALU.add, [PB[b], "ln_gb", "Cg"], [sk])
            tt("dve", catT[:, 4 * hb:4 * hb + 4, tl * 128:(tl + 1) * 128], sgt[hb], uT[:, 4 * hb:4 * hb + 4, tl * 128:(tl + 1) * 128],
               ALU.mult, [sk, "uT"], [f"cat{tl}"])
    S.barrier()
    if stage == 12:
        A.top = markX + XW
        return dbg_exit(lambda a: catT[:, a, :], 8, [f"cat{t}" for t in range(8)])

    A.top = markX
    qT = A.bf16(8 * T).rearrange("p (a b) -> p a b", b=T)
    kT = A.bf16(TH)
    vtok = A.bf16(9 * 128).rearrange("p (a b) -> p a b", b=128)
    kbd = A.bf16(8 * 512).rearrange("p (a b) -> p a b", b=512)
    sc = [A.f32(2048).rearrange("p (a b) -> p a b", b=256) for _ in range(3)]
    pn = [A.bf16(2048).rearrange("p (a b) -> p a b", b=256) for _ in range(2)]
    pT = [A.bf16(2048).rearrange("p (a b) -> p a b", b=128) for _ in range(2)]
    sm = [A.f32(64).rearrange("p (a b) -> p a b", b=8) for _ in range(3)]
    assert A.top <= markX + XW
    for c in range(8):
        wb, wk = get_w()
        for th in range(2):
            b = nextpb()
            for kc in range(KC):
                mm(pb[b][:, :], wb[:, kc, :], hT[:, kc, 128 + th * 512:128 + (th + 1) * 512], kc == 0, kc == KC - 1,
                   [wk, f"hT{kc}"], [PB[b]])
            ts("dve", qT[:, c, th * 512:(th + 1) * 512], pb[b][:, :], 0.125, ALU.mult, [PB[b]], ["qT"])
    wb, wk = get_w()
    for bi, (c0, cn) in enumerate(CB):
        b = nextpb()
        for kc in range(KC):
            mm(pb[b][:, 0:cn], wb[:, kc, :], hT[:, kc, c0:c0 + cn], kc == 0, kc == KC - 1, [wk, f"hT{kc}"], [PB[b]])
        cp("act", kT[:, c0:c0 + cn], pb[b][:, 0:cn], [PB[b]], ["kT"])
    wb, wk = get_w()
    for tl in range(9):
        b = nextpb()
        for kc in range(KC):
            mm(pb[b][:, 0:128], hT[:, kc, tl * 128:(tl + 1) * 128], wb[:, kc, :], kc == 0, kc == KC - 1,
               [wk, f"hT{kc}"], [PB[b]])
        cp("act", vtok[:, tl, :], pb[b][:, 0:128], [PB[b]], ["vtok"])
    S.op("dve", lambda E: E.memset(kbd, 0.0), [], ["kbd"])
    for blk in range(8):
        cp("dve", kbd[0:64, blk, 0:256], kT[0:64, blk * 128:blk * 128 + 256], ["kT"], ["kbd"])
        cp("act", kbd[64:128, blk, 256:512], kT[64:128, blk * 128:blk * 128 + 256], ["kT"], ["kbd"])
    if stage == 131:
        S.barrier()
        A.top = markX + XW
        return dbg_exit(lambda a: qT[:, a, :], 8, ["qT"])
    sink_v = sink_bc.rearrange("p (s c) -> p c s", s=2)
    iters = [(blk, c4) for blk in range(8) for c4 in range(2)]

    def att_s1a(n):
        blk, c4 = iters[n]
        i3 = n % 3
        mb = 0 if blk == 0 else 1
        for c_ in range(4):
            mm(pb[c_], qT[:, c4 * 4 + c_, blk * 128:(blk + 1) * 128], kbd[:, blk, :], True, True, ["qT", "kbd"], [PB[c_]])
        m_ = sm[i3]
        mk = f"sm{i3}"
        snk = sink_v[:, c4 * 4:c4 * 4 + 4, :]
        tt("dve", sc[i3], psA[:, 0:2048].rearrange("p (a b) -> p a b", b=256), amask[:, mb:mb + 1, :].broadcast_to([128, 8, 256]),
           ALU.add, PB[0:4] + ["amask"], [f"sc{i3}"])
        red("dve", m_[:, 0, :], sc[i3], ALU.max, [f"sc{i3}"], [mk])
        tt("dve", m_[:, 1, :].rearrange("p (c s) -> p c s", s=2), m_[:, 0, :].rearrange("p (c s) -> p c s", s=2), snk, ALU.max,
           [mk, "sink_bc"], [mk])
        ts("dve", m_[:, 2, :], m_[:, 1, :], -1.0, ALU.mult, [mk], [mk])
        tt("dve", m_[:, 4, :].rearrange("p (c s) -> p c s", s=2), snk, m_[:, 1, :].rearrange("p (c s) -> p c s", s=2), ALU.subtract,
           [mk, "sink_bc"], [mk])

    def att_s1b(n):
        blk, c4 = iters[n]
        i3 = n % 3
        i2 = n % 2
        m_ = sm[i3]
        mk = f"sm{i3}"
        for h in range(8):
            act(sc[i3][:, h, :], sc[i3][:, h, :], AF.Exp, [f"sc{i3}", mk], [f"sc{i3}", mk],
                bias=m_[:, 2, h:h + 1], scale=1.0, accum_out=m_[:, 3, h:h + 1])
        act(m_[:, 5, :], m_[:, 4, :], AF.Exp, [mk], [mk])
        tt("dve", m_[:, 6, :], m_[:, 5, :], m_[:, 3, :], ALU.add, [mk], [mk])
        S.op("dve", lambda E, m_=m_: E.reciprocal(out=m_[:, 7, :], in_=m_[:, 6, :]), [mk], [mk])
        tt("pool", pn[i2], sc[i3], m_[:, 7, :, None].broadcast_to([128, 8, 256]), ALU.mult, [f"sc{i3}", mk], [f"pn{i2}"])

    def att_s2(n):
        blk, c4 = iters[n]
        i2 = n % 2
        ptv = psA[:, 2048:3072].bitcast(BF16).rearrange("p (a b) -> p a b", b=128)
        for h in range(8):
            for kc2 in range(2):
                tr(ptv[:, h * 2 + kc2, :], pn[i2][:, h, kc2 * 128:(kc2 + 1) * 128], ident_b, [f"pn{i2}", "ident_b"], [PB[4 + h // 4]])
        cp("act", pT[i2], ptv, [PB[4], PB[5]], [f"pT{i2}"])
        for c_ in range(4):
            for s_ in range(2):
                lo, hi = s_ * 64, (s_ + 1) * 64
                for kc2 in range(2):
                    mm(pb[6][lo:hi, c_ * 128:(c_ + 1) * 128], vtok[:, blk + kc2, lo:hi], pT[i2][:, (c_ * 2 + s_) * 2 + kc2, :],
                       kc2 == 0, kc2 == 1, ["vtok", f"pT{i2}"], [PB[6]])
        cp("dve", catT[:, 8 + c4 * 4:8 + c4 * 4 + 4, blk * 128:(blk + 1) * 128], pb[6].rearrange("p (a b) -> p a b", b=128),
           [PB[6]], [f"cat{blk}"])

    NI = len(iters)
    att_s1a(0)
    att_s1a(1)
    att_s1b(0)
    for n in range(NI):
        if n + 2 < NI:
            att_s1a(n + 2)
        if n + 1 < NI:
            att_s1b(n + 1)
        att_s2(n)
    S.barrier()
    if stage == 13:
        A.top = markX + XW
        return dbg_exit(lambda a: catT[:, a, :], 16, [f"cat{t}" for t in range(8)])

    A.top = markX
    x1T = A.f32(KC * T).rearrange("p (a b) -> p a b", b=T)
    H2P_OFF = mark0 + 40256 + 5920 + 1024
    assert H2P_OFF >= markX + XW and H2P_OFF + 4096 <= A.words
    h2p0 = A.t[:, H2P_OFF:H2P_OFF + 4096].bitcast(BF16).rearrange("p (a b) -> p a b", b=512)
    CAT = [f"cat{t}" for t in range(8)]
    HT = [f"hT{k}" for k in range(KC)]

    def norm2_acc(kc):
        act(hT[:, kc, 0:T], x1T[:, kc, :], AF.Square, [f"x1T{kc}"], [f"hT{kc}"])
        for th in range(2):
            mm(pb[th][:, :], ones_b, hT[:, kc, th * 512:(th + 1) * 512], kc == 0, kc == KC - 1, ["ones_b", f"hT{kc}"], [PB[th]])
        dma("sp", "sp1", x1s_d[:, kc, :], x1T[:, kc, :], [f"x1T{kc}"], ["x1s"])
    xi = 0
    for dc in range(16):
        wb, wk = get_w()
        for th in range(2):
            b = nextpb()
            xs_ = xstg[xi % 2]
            xk = f"xstg{xi % 2}"
            xi += 1
            dma("sp", xk, xs_, xT_d[:, dc, 128 + th * 512:128 + (th + 1) * 512], [], [xk])
            for fc in range(KC):
                mm(pb[b][:, :], wb[:, fc, :], catT[:, fc, th * 512:(th + 1) * 512], fc == 0, fc == KC - 1,
                   [wk] + CAT[4 * th:4 * th + 4], [PB[b]])
            tt("dve", x1T[:, dc, th * 512:(th + 1) * 512], xs_, pb[b][:, :], ALU.add, [PB[b], xk], [f"x1T{dc}"])
        if dc > 0:
            norm2_acc(dc - 1)
    norm2_acc(KC - 1)
    for th in range(2):
        ts("dve", rstd[:, th * 512:(th + 1) * 512], pb[th][:, :], 1.0 / D, ALU.mult, [PB[th]], ["rstd"], s2=EPS, op1=ALU.add)
    act(rstd[:, 0:T], rstd[:, 0:T], AF.Sqrt, ["rstd"], ["rstd"])
    S.op("dve", lambda E: E.reciprocal(out=rstd[:, 0:T], in_=rstd[:, 0:T]), ["rstd"], ["rstd"])
    for kc in range(KC):
        stt("dve", h2p0[:, kc, :], x1T[:, kc, 0:512], gvec[:, 1, kc:kc + 1], rstd[:, 0:512], ALU.mult, ALU.mult,
            [f"x1T{kc}", "gvec", "rstd"], ["h2p"])
    for kc in range(KC):
        stt("dve", hT[:, kc, 512:T], x1T[:, kc, 512:T], gvec[:, 1, kc:kc + 1], rstd[:, 512:T], ALU.mult, ALU.mult,
            [f"x1T{kc}", "gvec", "rstd"], [f"hT{kc}"])
        if kc % 4 == 3:
            dma("sp", "sp2", h2s_d[:, kc - 3:kc + 1, :], hT[:, kc - 3:kc + 1, 512:T], HT[kc - 3:kc + 1], ["h2s"])
    S.barrier()

    TP = 512
    A.top = mark0
    Wb = A.bf16(128 * TP).rearrange("p (j t) -> p j t", t=TP)
    rank = A.f32(4 * TP).rearrange("p (a t) -> p a t", t=TP)
    q2T = A.bf16(8 * TP).rearrange("p (h t) -> p h t", t=TP)
    IfTb = A.bf16(TP)
    iota_b = A.bf16(128)
    cp("dve", iota_b, iota_f, ["cst"], ["iota_b"])
    wb2_off = A.top
    wbuf2 = [A.bf16(KC * 128).rearrange("p (a b) -> p a b", b=128) for _ in range(NWB)]
    WB2K = [f"wbuf2_{i}" for i in range(NWB)]
    markZ = A.top
    NWQ = NWB + 1
    WQ3_OFF = mark0 + 40256 + 5920 + 1024 + 4096
    assert WQ3_OFF + 1024 <= A.words
    wbuf2.append(A.t[:, WQ3_OFF:WQ3_OFF + 1024].bitcast(BF16).rearrange("p (a b) -> p a b", b=128))
    wq_srcs = [w_qr_d[cc] for cc in range(16)] * 2
    wq_state = {"issued": 0, "used": 0}

    def get_wq():
        lim = (wq_state["used"] // 16 + 1) * 16
        while wq_state["issued"] < min(lim, wq_state["used"] + NWQ):
            n = wq_state["issued"]
            i = n % NWQ
            dma("pool", f"wq{i}", wbuf2[i], wq_srcs[n], [], [f"wbuf2_{i}"])
            wq_state["issued"] += 1
        i = wq_state["used"] % NWQ
        wq_state["used"] += 1
        return wbuf2[i], f"wbuf2_{i}"

    x2T_e = A.t[:, mark0:mark0 + KC * T].rearrange("p (a b) -> p a b", b=T)
    for tp in range(2):
        tok0 = tp * TP
        A.top = markZ
        ND = 4
        dwb = [A.bf16(KC * 128).rearrange("p (a b) -> p a b", b=128) for _ in range(ND)]
        q1T = A.bf16(8 * TP).rearrange("p (h t) -> p h t", t=TP)
        cand = A.f32(256).rearrange("p (r c) -> p r c", c=16)
        sm8 = A.f32(32).rearrange("p (a h) -> p a h", h=8)
        rk = A.f32(512).rearrange("p (a h r) -> p a h r", h=8, r=16)
        assert A.top == H2P_OFF, (A.top, H2P_OFF)
        h2p = A.bf16(KC * TP).rearrange("p (a b) -> p a b", b=TP)
        scr = A.t[:, wb2_off:wb2_off + 3072]
        o_ = {"n": 0}

        def sf32(n):
            off = o_["n"]
            o_["n"] += (n + 7) // 8 * 8
            assert o_["n"] <= 3072
            return scr[:, off:off + n]
        s12 = sf32(2048).rearrange("p (a b) -> p a b", b=128)
        wk1 = sf32(128)
        wk2 = sf32(256)
        v1 = sf32(128).rearrange("p (h r) -> p h r", r=16)
        v2 = sf32(128).rearrange("p (h r) -> p h r", r=16)
        I1 = sf32(128).bitcast(U32).rearrange("p (h r) -> p h r", r=16)
        tv = sf32(128).rearrange("p (h r) -> p h r", r=16)
        ev = sf32(128).rearrange("p (h r) -> p h r", r=16)

        dst = {"issued": 0}

        def get_d(j):
            while dst["issued"] < min(128, j + ND):
                n = dst["issued"]
                dma("pool", f"dw{n % ND}", dwb[n % ND], dwn_d[n], [], [f"dwb{n % ND}"])
                dst["issued"] += 1
            return dwb[j % ND], f"dwb{j % ND}"

        for cc in range(16):
            wb, wk = get_wq()
            b = nextpb(4, 8)
            for kc in range(KC):
                mm(pb[b][:, :], wb[:, kc, :], h2p[:, kc, :], kc == 0, kc == KC - 1, [wk, "h2p"], [PB[b]])
            h, half = cc // 2, cc % 2
            if half == 0:
                cp("act", q1T[:, h, :], pb[b][:, :], [PB[b]], ["q1T"])
            else:
                cp("dve", q2T[:, h, :], pb[b][:, :], [PB[b]], ["q2T"])
            if cc == 11:
                get_d(0)

        def prepA(tl):
            tc0 = tl * 128
            for cc in range(16):
                h, half = cc // 2, cc % 2
                src = q1T if half == 0 else q2T
                mm(pb[4 + cc // 4][:, (cc % 4) * 128:(cc % 4 + 1) * 128], src[:, h, tc0:tc0 + 128], keysT[:, half, :], True, True,
                   ["q1T", "q2T", "keysT"], [PB[4 + cc // 4]])
            for b4 in range(4):
                cp("dve", s12[:, 4 * b4:4 * b4 + 4, :], pb[4 + b4][:, :].rearrange("p (a b) -> p a b", b=128), [PB[4 + b4]], ["s12"] + WB2K)
            for h in range(8):
                a1 = s12[:, 2 * h, :]
                a2 = s12[:, 2 * h + 1, :]
                S.op("dve", lambda E, a1=a1, h=h: E.max(out=v1[:, h, 0:8], in_=a1), ["s12"], ["v1"])
                S.op("dve", lambda E, a1=a1, h=h: E.max_index(out=I1[:, h, 0:8], in_max=v1[:, h, 0:8], in_values=a1), ["s12", "v1"], ["I1"])
                S.op("dve", lambda E, a1=a1, h=h: E.match_replace(out=wk1, in_to_replace=v1[:, h, 0:8], in_values=a1, imm_value=-1e30), ["s12", "v1"], ["wk1"])
                S.op("dve", lambda E, h=h: E.max(out=v1[:, h, 8:16], in_=wk1), ["wk1"], ["v1"])
                S.op("dve", lambda E, a1=a1, h=h: E.max_index(out=I1[:, h, 8:16], in_max=v1[:, h, 8:16], in_values=a1), ["s12", "v1"], ["I1"])
                S.op("dve", lambda E, a2=a2, h=h: E.max(out=v2[:, h, 0:8], in_=a2), ["s12"], ["v2"])
                S.op("dve", lambda E, a2=a2, h=h: E.match_replace(out=wk1, in_to_replace=v2[:, h, 0:8], in_values=a2, imm_value=-1e30), ["s12", "v2"], ["wk1"])
                S.op("dve", lambda E, h=h: E.max(out=v2[:, h, 8:16], in_=wk1), ["wk1"], ["v2"])
            ch = cand.rearrange("p r c -> p (r c)")
            for h in range(8):
                tt("dve", cand, v1[:, h, :, None].broadcast_to([128, 16, 16]), v2[:, h, None, :].broadcast_to([128, 16, 16]), ALU.add,
                   ["v1", "v2"], ["cand"])
                S.op("dve", lambda E, h=h: E.max(out=tv[:, h, 0:8], in_=ch), ["cand"], ["tv"])
                S.op("dve", lambda E, h=h: E.match_replace(out=wk2, in_to_replace=tv[:, h, 0:8], in_values=ch, imm_value=-1e30), ["cand", "tv"], ["wk2"])
                S.op("dve", lambda E, h=h: E.max(out=tv[:, h, 8:16], in_=wk2), ["wk2"], ["tv"])
            tt("dve", ev, tv, tv[:, :, 0:1].broadcast_to([128, 8, 16]), ALU.subtract, ["tv"], ["ev"])

        def prepB(tl):
            act(ev, ev, AF.Exp, ["ev"], ["ev"])
            red("dve", sm8[:, 0, :], ev, ALU.add, ["ev"], ["sm8"])
            ts("dve", sm8[:, 2, :], tv[:, :, 15], -1e-4, ALU.add, ["tv"], ["sm8"])
            tt("dve", rk[:, 0, :, :], sm8[:, 2, :, None].broadcast_to([128, 8, 16]), v1, ALU.subtract, ["sm8", "v1"], ["rk"])
            act(sm8[:, 3, :], sm8[:, 0, :], AF.Ln, ["sm8"], ["sm8"])
            tt("dve", sm8[:, 3, :], sm8[:, 3, :], v2[:, :, 0], ALU.add, ["sm8", "v2"], ["sm8"])
            tt("dve", sm8[:, 3, :], sm8[:, 3, :], v1[:, :, 0], ALU.add, ["sm8", "v1"], ["sm8"])
            tt("dve", rk[:, 3, :, :], v1, sm8[:, 3, :, None].broadcast_to([128, 8, 16]), ALU.subtract, ["v1", "sm8"], ["rk"])
            cp("dve", rk[:, 2, :, :], I1, ["I1"], ["rk"])
            cp("dve", rk[:, 1, :, :], I1, ["I1"], ["rk"])

        def prepC(tl):
            tc0 = tl * 128
            bT = 4 + tl % 2
            for a in range(4):
                tr(pb[bT][:, a * 128:(a + 1) * 128], rk[:, a, :, :].rearrange("p h r -> p (h r)"), ident_f, ["rk", "cst"], [PB[bT]])
            cp("dve", rank[:, :, tc0:tc0 + 128], pb[bT][:, :].rearrange("p (a b) -> p a b", b=128), [PB[bT]], ["rank"])
            cp("dve", IfTb[:, tc0:tc0 + 128], pb[bT][:, 256:384], [PB[bT]], ["rank"])

        hooks = {}
        for tl in range(4):
            j0 = 2 + 31 * tl
            hooks[j0] = (prepA, tl)
            hooks[j0 + 16] = (prepB, tl)
            hooks[j0 + 24] = (prepC, tl)

        for j in range(128):
            dw, dk = get_d(j)
            b = nextpb(0, 4)
            for kc in range(KC):
                mm(pb[b][:, :], dw[:, kc, :], h2p[:, kc, :], kc == 0, kc == KC - 1, [dk, "h2p"], [PB[b]])
            act(Wb[:, j, :], pb[b][:, :], AF.Gelu, [PB[b]], [f"W{j // 16}"])
            if j in hooks:
                fn_, tl_ = hooks[j]
                fn_(tl_)
        S.barrier()
        if stage == 21:
            S.finish()
            es.close()
            return nc

        A.top = markZ
        NB = 8
        q2rep = [A.bf16(NB * 128).rearrange("p (t m) -> p t m", m=128) for _ in range(2)]
        Lb = [A.bf16(NB * 128).rearrange("p (t m) -> p t m", m=128) for _ in range(2)]
        Rb = [A.bf16(NB * 128).rearrange("p (t m) -> p t m", m=128) for _ in range(2)]
        ebb = [A.bf16(NB * 128).rearrange("p (t m) -> p t m", m=128) for _ in range(2)]
        mskb = [A.bf16(NB * 128).rearrange("p (t m) -> p t m", m=128) for _ in range(2)]
        WK = [f"W{k}" for k in range(8)]
        nbat = TP // NB

        def pAv(i2):
            return psA[:, i2 * 1024:(i2 + 1) * 1024].rearrange("p (t m) -> p t m", m=128), [PB[2 * i2], PB[2 * i2 + 1]]

        def pGv(i2):
            return psA[:, 2048 + i2 * 1024:2048 + (i2 + 1) * 1024].rearrange("p (t m) -> p t m", m=128), [PB[4 + 2 * i2], PB[5 + 2 * i2]]

        def frontA1(tb):
            t0 = tb * NB
            i2 = tb % 2
            pA, kA = pAv(i2)
            cp("act", q2rep[i2].rearrange("p t (h r) -> p t h r", r=16),
               q2T[:, :, t0:t0 + NB].rearrange("p h t -> p t h")[:, :, :, None].broadcast_to([128, NB, 8, 16]), ["q2T"], [f"q2rep{i2}"])

        def frontA1mm(tb):
            i2 = tb % 2
            pA, kA = pAv(i2)
            for k in range(NB):
                mm(pA[:, k, :], q2rep[i2][:, k, :], keysT[:, 1, :], True, True, [f"q2rep{i2}", "keysT"], [kA[k // 4]])

        def frontA2(tb):
            t0 = tb * NB
            i2 = tb % 2
            pA, kA = pAv(i2)
            H = NB // 2

            def exps(hf):
                for k in range(hf * H, (hf + 1) * H):
                    act(ebb[i2][:, k, :], pA[:, k, :], AF.Exp, ["rank"], [f"eb{i2}{hf}", kA[hf]], bias=rank[:, 3, t0 + k:t0 + k + 1], scale=1.0)

            def mask(hf):
                tt("dve", mskb[i2][:, hf * H:(hf + 1) * H, :], pA[:, hf * H:(hf + 1) * H, :],
                   rank[:, 0, t0 + hf * H:t0 + (hf + 1) * H, None].broadcast_to([128, H, 128]), ALU.is_ge, ["rank"], [f"msk{i2}{hf}", kA[hf]])
            mask(0)
            exps(1)
            tt("dve", Lb[i2], iota_b[:, None, :].broadcast_to([128, NB, 128]), IfTb[:, t0:t0 + NB, None].broadcast_to([128, NB, 128]),
               ALU.is_equal, ["iota_b", "rank"], [f"L{i2}"])
            mask(1)
            exps(0)
            for hf in (1, 0):
                tt("pool", Rb[i2][:, hf * H:(hf + 1) * H, :], mskb[i2][:, hf * H:(hf + 1) * H, :], ebb[i2][:, hf * H:(hf + 1) * H, :], ALU.mult,
                   [f"msk{i2}{hf}", f"eb{i2}{hf}"], [f"R{i2}{hf}"])

        def backGmm(tb):
            i2 = tb % 2
            pG, kG = pGv(i2)
            H = NB // 2
            for k in list(range(H, NB)) + list(range(0, H)):
                mm(pG[:, k, :], Lb[i2][:, k, :], Rb[i2][:, k, :], True, True, [f"L{i2}", f"R{i2}{k // H}"], [kG[k // 4]])

        def backW(tb):
            t0 = tb * NB
            i2 = tb % 2
            pG, kG = pGv(i2)
            wv = Wb[:, :, t0:t0 + NB]
            tt("dve", wv, pG.rearrange("p t j -> p j t"), wv, ALU.mult, kG + WK, WK)

        frontA1(0)
        frontA1mm(0)
        if nbat > 1:
            frontA1(1)
            frontA1mm(1)
        frontA2(0)
        for tb in range(nbat):
            if tb + 2 < nbat:
                frontA1(tb + 2)
            backGmm(tb)
            if tb + 2 < nbat:
                frontA1mm(tb + 2)
            if tb + 1 < nbat:
                frontA2(tb + 1)
            backW(tb)
        S.barrier()
        if stage == 22:
            S.finish()
            es.close()
            return nc

        A.top = wb2_off
        NUB = 5
        xs3 = [A.f32(512) for _ in range(8)]
        upb = [A.bf16(2 * 1024).rearrange("p (a b) -> p a b", b=1024) for _ in range(NUB)]
        assert A.top <= H2P_OFF
        if tp == 0:
            dma("sp", "h2p", h2p, h2s_d, ["h2s"], ["h2p"])
        for dh in range(2):
            ust = {"issued": 0}

            def get_u(jp, dh=dh, ust=ust):
                while ust["issued"] < min(64, jp + NUB):
                    n = ust["issued"]
                    dma("pool", f"up{n % NUB}", upb[n % NUB], upw_d[dh, 2 * n:2 * n + 2].rearrange("a i n -> i a n"), [], [f"upb{n % NUB}"])
                    ust["issued"] += 1
                return upb[jp % NUB], f"upb{jp % NUB}"
            for dc in range(8):
                dma("sp", "xs3l", xs3[dc], x1s_d[:, dh * 8 + dc, tok0:tok0 + TP], ["x1s"], [f"xs3{dc}"])
            last = (tp == 1 and dh == 1)
            for j in range(128):
                ub, uk = get_u(j // 2)
                for dc in range(8):
                    mm(pb[dc], ub[:, j % 2, dc * 128:(dc + 1) * 128], Wb[:, j, :], j == 0, j == 127, [uk, f"W{j // 16}"], [PB[dc]])
                if last and j == 64:
                    WD = ["W0", "W1", "W2", "W3"]
                    for i in range(2):
                        dma("act", "x2l", x2T_e[:, 4 * i:4 * i + 4, :], x1s_d[:, 4 * i:4 * i + 4, :], ["x2s", "x1s"],
                            [f"x2T{k}" for k in range(4 * i, 4 * i + 4)] + WD)
                    for i in range(2, 4):
                        dma("act", "x2l", x2T_e[:, 4 * i:4 * i + 4, 0:TP], x1s_d[:, 4 * i:4 * i + 4, 0:TP], ["x2s", "x1s"],
                            [f"x2T{k}" for k in range(4 * i, 4 * i + 4)] + WD)
            for dc in range(8):
                kc = dh * 8 + dc
                if last:
                    tt("dve", x2T_e[:, kc, tok0:tok0 + TP], xs3[dc], pb[dc], ALU.add, [PB[dc], f"xs3{dc}"], [f"x2T{kc}"])
                else:
                    tt("dve", xs3[dc], xs3[dc], pb[dc], ALU.add, [PB[dc], f"xs3{dc}"], [f"xs3{dc}"])
                    dma("sp", "xo3", x1s_d[:, kc, tok0:tok0 + TP], xs3[dc], [f"xs3{dc}"], ["x2s"])
        S.barrier()

    A.top = mark0
    x2T = A.f32(KC * T).rearrange("p (a b) -> p a b", b=T)
    sq2 = A.bf16(KC * T).rearrange("p (a b) -> p a b", b=T)
    rs3 = A.f32(T)
    assert A.top - KC * T - KC * T // 2 - T == mark0
    for kc in range(KC):
        if kc % 3 == 2:
            tt("dve", sq2[:, kc, :], x2T[:, kc, :], x2T[:, kc, :], ALU.mult, [f"x2T{kc}"], [f"sq2{kc}"])
        else:
            act(sq2[:, kc, :], x2T[:, kc, :], AF.Square, [f"x2T{kc}"], [f"sq2{kc}"])
        for th in range(2):
            mm(pb[th][:, :], ones_b, sq2[:, kc, th * 512:(th + 1) * 512], kc == 0, kc == KC - 1, ["ones_b", f"sq2{kc}"], [PB[th]])
    for th in range(2):
        ts("dve", rs3[:, th * 512:(th + 1) * 512], pb[th][:, :], 1.0 / D, ALU.mult, [PB[th]], ["rs3"], s2=EPS, op1=ALU.add)
    act(rs3, rs3, AF.Sqrt, ["rs3"], ["rs3"])
    S.op("dve", lambda E: E.reciprocal(out=rs3, in_=rs3), ["rs3"], ["rs3"])
    for kc in range(KC):
        stt("dve", x2T[:, kc, :], x2T[:, kc, :], gvec[:, 2, kc:kc + 1], rs3, ALU.mult, ALU.mult, [f"x2T{kc}", "gvec", "rs3"], [f"x2T{kc}"])
    for i in range(4):
        dma("sp" if i % 2 == 0 else "act", "y", yT_d[:, 4 * i:4 * i + 4, :], x2T[:, 4 * i:4 * i + 4, :], [f"x2T{k}" for k in range(4 * i, 4 * i + 4)], ["y"])

    S.finish()
    es.close()
    return nc


def _prep(inputs):
    f = np.float32
    x = np.asarray(inputs["x"], f)
    w_in = np.asarray(inputs["w_in"], f)[0]
    common = {}

    def chunked(w, ncol_chunks):
        return np.ascontiguousarray(w.reshape(KC, 128, ncol_chunks, 128).transpose(2, 1, 0, 3))

    common["w_u"] = chunked(w_in[:, 0:1024], 8)
    common["w_v"] = np.ascontiguousarray(w_in[:, 1024:2048].reshape(KC, 128, 1024).transpose(1, 0, 2))
    wq = w_in[:, 2048:3072].reshape(D, 16, 64)
    wq_perm = np.stack([wq[:, 0:8], wq[:, 8:16]], axis=2).reshape(D, 1024)
    common["w_q"] = chunked(wq_perm, 8)
    common["w_k"] = np.ascontiguousarray(w_in[:, 3072:3200].reshape(KC, 128, 128).transpose(1, 0, 2))
    common["w_vv"] = np.ascontiguousarray(w_in[:, 3200:3328].reshape(KC, 128, 128).transpose(1, 0, 2))
    gv = np.stack([np.asarray(inputs["norm1_g"], f)[0], np.asarray(inputs["norm2_g"], f)[0], np.asarray(inputs["norm_f_g"], f)], 0)
    common["gvec"] = np.ascontiguousarray(gv.reshape(3, KC, 128).transpose(2, 0, 1))
    common["wsT"] = np.ascontiguousarray(np.asarray(inputs["w_spatial"], f)[0].transpose(2, 0, 1))
    common["bs_bc"] = np.ascontiguousarray(np.broadcast_to(np.asarray(inputs["b_spatial"], f)[0][None], (128, 8, 128)))
    common["sink_bc"] = np.ascontiguousarray(np.broadcast_to(np.asarray(inputs["attn_sinks"], f)[0][None], (128, 16)))
    common["ln_gb"] = np.ascontiguousarray(np.stack([np.asarray(inputs["sgu_ln_g"], f)[0].T, np.asarray(inputs["sgu_ln_b"], f)[0].T], 1))
    w_out = np.asarray(inputs["w_out"], f)[0]
    wo_a = w_out[0:1024]
    wo_b = w_out[1024:2048].reshape(16, 64, D)
    wo_bp = np.stack([wo_b[0:8], wo_b[8:16]], axis=1).reshape(1024, D)
    common["w_o"] = chunked(np.concatenate([wo_a, wo_bp], 0), 16)
    common["w_qr"] = chunked(np.asarray(inputs["w_query"], f)[0], 16)
    common["keysT"] = np.ascontiguousarray(np.asarray(inputs["sub_keys"], f)[0].transpose(2, 0, 1))
    ed = np.asarray(inputs["expert_down"], f)[0]
    common["dwn"] = np.ascontiguousarray(ed.reshape(128, 128, KC, 128).transpose(1, 3, 2, 0))
    eu = np.asarray(inputs["expert_up"], f)[0]
    common["upw"] = np.ascontiguousarray(eu.reshape(128, 128, 2, 1024).transpose(2, 1, 0, 3))
    ident = np.eye(128, dtype=f)
    iota = np.broadcast_to(np.arange(128, dtype=f)[None], (128, 128))
    cm = (np.arange(128)[:, None] <= np.arange(128)[None, :]).astype(f)
    common["cst"] = np.ascontiguousarray(np.stack([ident, iota, cm], 1))
    i = np.arange(128)[:, None]
    j = np.arange(256)[None, :]
    band = (j >= i + 1) & (j <= i + 128)
    m_all = np.where(band, 0.0, NEG).astype(f)
    m_first = np.where(band & (j >= 128), 0.0, NEG).astype(f)
    in_maps = []
    for c in range(NCORES):
        b, half = c // 2, c % 2
        t0 = half * T
        xs = np.zeros((TH, D), f)
        xs[128:] = x[b, t0:t0 + T]
        if half == 1:
            xs[:128] = x[b, t0 - 128:t0]
        m = dict(common)
        m["xT"] = np.ascontiguousarray(xs.T.reshape(KC, 128, TH).transpose(1, 0, 2))
        m["amask"] = np.ascontiguousarray(np.stack([m_first if half == 0 else m_all, m_all], 1))
        in_maps.append(m)
    return in_maps


def _gather(res):
    y = np.empty((4, 2048, D), np.float32)
    for c in range(NCORES):
        b, half = c // 2, c % 2
        yT = np.asarray(res.results[c]["yT"])
        y[b, half * T:(half + 1) * T] = yT.transpose(2, 1, 0).reshape(T, D)
    return y


def kernel(**inputs):
    in_maps = _prep(inputs)
    nc = build()
    res = run_bass_kernel_spmd(nc, in_maps, core_ids=list(range(NCORES)))
    return _gather(res)
```

```python
import contextlib
import numpy as np
import concourse.bass as bass
import concourse.mybir as mybir
from concourse.bass_utils import run_bass_kernel_spmd

F32 = mybir.dt.float32
BF16 = mybir.dt.bfloat16
U32 = mybir.dt.uint32
AF = mybir.ActivationFunctionType
ALU = mybir.AluOpType
AX = mybir.AxisListType

NCORES = 8
T = 1024
TH = 1152
D = 2048
KC = 16
EPS = 1e-6
NEG = -30000.0


_NEED = None


class Sched:
    ENGS = ("pe", "act", "dve", "pool", "sp")
    last_need = None

    def __init__(self, nc):
        import bisect
        self._bisect = bisect
        self.nc = nc
        self.dry = _NEED is None
        self.need = {e: set() for e in ("pe", "act", "dve", "pool")} if self.dry else None
        self.rank = None if self.dry else {e: sorted(v) for e, v in _NEED.items()}
        self.needset = None if self.dry else _NEED
        self.streams = {e: [] for e in self.ENGS}
        self.cnt = {}
        self.semh = {}
        self.waited = {e: {} for e in self.ENGS}
        self.last_w = {}
        self.readers = {}
        self._ctx = []
        for e in ("pe", "act", "dve", "pool"):
            self._mk_sem(e)

    def _mk_sem(self, name):
        cm = self.nc.semaphore("s_" + name)
        h = cm.__enter__()
        self._ctx.append(cm)
        self.semh[name] = h
        self.cnt[name] = 0

    def _val(self, dom, c):
        if dom.startswith("d_"):
            return c
        if self.dry:
            self.need[dom].add(c)
            return c
        return self._bisect.bisect_right(self.rank[dom], c)

    def _deps(self, eng, reads, writes):
        deps = {}

        def add(dom, c):
            if dom.startswith("d_"):
                c = self.cnt[dom]
            deps[dom] = max(deps.get(dom, 0), c)
        for k in reads:
            lw = self.last_w.get(k)
            if lw:
                add(*lw)
        for k in writes:
            lw = self.last_w.get(k)
            if lw and lw[0] != eng:
                add(*lw)
            for dom, c in self.readers.get(k, {}).items():
                if dom != eng:
                    add(dom, c)
        out = []
        for dom, c in deps.items():
            if self.waited[eng].get(dom, 0) < c:
                self.waited[eng][dom] = c
                out.append((dom, self._val(dom, c)))
        return out

    def _record(self, dom, my, reads, writes):
        for k in reads:
            self.readers.setdefault(k, {})[dom] = my
        for k in writes:
            self.last_w[k] = (dom, my)
            self.readers[k] = {}

    def op(self, eng, fn, reads=(), writes=()):
        waits = self._deps(eng, reads, writes)
        self.cnt[eng] += 1
        my = self.cnt[eng]
        sem = self.semh[eng]
        semh = self.semh
        sig = self.dry or (my in self.needset[eng])

        def thunk(E):
            for dom, v in waits:
                E.wait_ge(semh[dom], v)
            ins = fn(E)
            if sig:
                ins.then_inc(sem, 1)
        self.streams[eng].append(thunk)
        self._record(eng, my, reads, writes)

    def dma(self, q, dsem, fn, reads=(), writes=()):
        dom = "d_" + dsem
        if dom not in self.semh:
            self._mk_sem(dom)
        waits = self._deps(q, reads, writes)
        self.cnt[dom] += 16
        my = self.cnt[dom]
        sem = self.semh[dom]
        semh = self.semh

        def thunk(E):
            for d, v in waits:
                E.wait_ge(semh[d], v)
            fn(E).then_inc(sem, 16)
        self.streams[q].append(thunk)
        self._record(dom, my, reads, writes)

    def barrier(self):
        snap = dict(self.cnt)
        semh = self.semh
        for e in self.ENGS:
            waits = []
            for dom, c in snap.items():
                if c > 0 and self.waited[e].get(dom, 0) < c:
                    self.waited[e][dom] = c
                    waits.append((dom, self._val(dom, c)))

            def thunk(E, waits=waits):
                for d, v in waits:
                    E.wait_ge(semh[d], v)
            self.streams[e].append(thunk)

    def finish(self):
        self.barrier()
        if self.dry:
            Sched.last_need = self.need
            for cm in reversed(self._ctx):
                cm.__exit__(None, None, None)
            return
        nc = self.nc
        streams = self.streams
        with nc.Block() as block:
            @block.tensor
            def _(E):
                for t in streams["pe"]:
                    t(E)

            @block.scalar
            def _(E):
                for t in streams["act"]:
                    t(E)

            @block.vector
            def _(E):
                for t in streams["dve"]:
                    t(E)

            @block.gpsimd
            def _(E):
                for t in streams["pool"]:
                    t(E)

            @block.sync
            def _(E):
                for t in streams["sp"]:
                    t(E)
        for cm in reversed(self._ctx):
            cm.__exit__(None, None, None)


class Arena:
    def __init__(self, nc, es, words):
        self.t = es.enter_context(nc.sbuf_tensor("arena", [128, words], F32))
        self.top = 0
        self.words = words

    def f32(self, n):
        n8 = (n + 7) // 8 * 8
        off = self.top
        self.top += n8
        assert self.top <= self.words, ("arena overflow", self.top, self.words)
        return self.t[:, off:off + n]

    def bf16(self, n):
        w = (n + 1) // 2
        w8 = (w + 7) // 8 * 8
        off = self.top
        self.top += w8
        assert self.top <= self.words, ("arena overflow", self.top, self.words)
        return self.t[:, off:off + w].bitcast(BF16)


def build(stage=99):
    global _NEED
    _NEED = None
    _build(stage)
    _NEED = Sched.last_need
    nc = _build(stage)
    _NEED = None
    return nc


def _build(stage=99):
    nc = bass.Bass("TRN2", target_bir_lowering=False)
    es = contextlib.ExitStack()

    def din(name, shape, dt=F32):
        return nc.dram_tensor(name, list(shape), dt, kind="ExternalInput").ap()

    xT_d = din("xT", [128, KC, TH])
    amask_d = din("amask", [128, 2, 256])
    w_u_d = din("w_u", [8, 128, KC, 128])
    w_v_d = din("w_v", [128, KC, 1024])
    w_q_d = din("w_q", [8, 128, KC, 128])
    w_k_d = din("w_k", [128, KC, 128])
    w_vv_d = din("w_vv", [128, KC, 128])
    gvec_d = din("gvec", [128, 3, KC])
    wsT_d = din("wsT", [128, 8, 128])
    bs_bc_d = din("bs_bc", [128, 8, 128])
    sink_bc_d = din("sink_bc", [128, 16])
    ln_d = din("ln_gb", [128, 2, 8])
    w_o_d = din("w_o", [16, 128, KC, 128])
    w_qr_d = din("w_qr", [16, 128, KC, 128])
    keysT_d = din("keysT", [128, 2, 128])
    if stage >= 20:
        dwn_d = din("dwn", [128, 128, KC, 128])
        if stage not in (21, 22):
            upw_d = din("upw", [2, 128, 128, 1024])
    cst_d = din("cst", [128, 3, 128])
    yT_d = nc.dram_tensor("yT", [128, KC, T], F32, kind="ExternalOutput").ap()
    x1s_d = nc.dram_tensor("x1s", [128, KC, T], F32, kind="ExternalOutput").ap()
    h2s_d = nc.dram_tensor("h2s", [128, KC, 512], BF16, kind="ExternalOutput").ap()

    S = Sched(nc)
    A = Arena(nc, es, 53200)
    psA = es.enter_context(nc.psum_tensor("psA", [128, 4096], F32))
    pb = [psA[:, i * 512:(i + 1) * 512] for i in range(8)]
    PB = [f"pb{i}" for i in range(8)]

    def mm(out, lhsT, rhs, start, stop, reads, writes):
        S.op("pe", lambda E: E.matmul(out, lhsT=lhsT, rhs=rhs, start=start, stop=stop), reads, writes)

    def tr(out, in_, ident, reads, writes):
        S.op("pe", lambda E: E.transpose(out, in_, ident), reads, writes)

    def act(out, in_, func, reads, writes, bias=None, scale=None, accum_out=None, eng="act"):
        kw = {}
        if bias is not None:
            kw["bias"] = bias
        if scale is not None:
            kw["scale"] = scale
        if accum_out is not None:
            kw["accum_out"] = accum_out
        S.op("act", lambda E: E.activation(out=out, in_=in_, func=func, **kw), reads, writes)

    def tt(eng, out, in0, in1, op, reads, writes):
        S.op(eng, lambda E: E.tensor_tensor(out=out, in0=in0, in1=in1, op=op), reads, writes)

    def ts(eng, out, in0, s1, op0, reads, writes, s2=None, op1=None):
        if op1 is None:
            S.op(eng, lambda E: E.tensor_scalar(out=out, in0=in0, scalar1=s1, scalar2=None, op0=op0), reads, writes)
        else:
            S.op(eng, lambda E: E.tensor_scalar(out=out, in0=in0, scalar1=s1, scalar2=s2, op0=op0, op1=op1), reads, writes)

    def stt(eng, out, in0, scalar, in1, op0, op1, reads, writes):
        S.op(eng, lambda E: E.scalar_tensor_tensor(out=out, in0=in0, scalar=scalar, in1=in1, op0=op0, op1=op1), reads, writes)

    def cp(eng, out, in_, reads, writes):
        if eng == "act":
            S.op("act", lambda E: E.activation(out=out, in_=in_, func=AF.Copy), reads, writes)
        else:
            S.op(eng, lambda E: E.tensor_copy(out, in_), reads, writes)

    def red(eng, out, in_, op, reads, writes):
        S.op(eng, lambda E: E.tensor_reduce(out=out, in_=in_, axis=AX.X, op=op), reads, writes)

    def dma(q, sem, out, in_, reads, writes):
        S.dma(q, sem, lambda E: E.dma_start(out=out, in_=in_), reads, writes)


    def dbg_exit(src_fn, nchunks, keys):
        stg = [A.f32(1024), A.f32(1024)]
        for a in range(nchunks):
            cp("dve", stg[a % 2], src_fn(a), keys, [f"dbg{a % 2}"])
            dma("sp", "y", yT_d[:, a, :], stg[a % 2], [f"dbg{a % 2}"], ["y"])
        S.finish()
        es.close()
        return nc

    cst = A.f32(3 * 128).rearrange("p (a b) -> p a b", b=128)
    ident_f = cst[:, 0, :]
    iota_f = cst[:, 1, :]
    cmask = cst[:, 2, :]
    ident_b = A.bf16(128)
    ones_b = A.bf16(128)
    gvec = A.f32(3 * KC).rearrange("p (a b) -> p a b", b=KC)
    keysT = A.bf16(2 * 128).rearrange("p (a b) -> p a b", b=128)
    dma("sp", "c0", cst, cst_d, [], ["cst"])
    dma("sp", "c0", gvec, gvec_d, [], ["gvec"])
    dma("pool", "c1", keysT, keysT_d, [], ["keysT"])
    cp("dve", ident_b, ident_f, ["cst"], ["ident_b"])
    S.op("dve", lambda E: E.memset(ones_b, 1.0), [], ["ones_b"])
    mark0 = A.top

    hT = A.bf16(KC * TH).rearrange("p (a b) -> p a b", b=TH)
    catT = A.bf16(16 * T).rearrange("p (a b) -> p a b", b=T)
    amask = A.f32(512).rearrange("p (a b) -> p a b", b=256)
    sink_bc = A.f32(16)
    ln_gb = A.f32(16).rearrange("p (a b) -> p a b", b=8)
    wsb = A.bf16(1024).rearrange("p (a b) -> p a b", b=128)
    Cg = A.f32(1024).rearrange("p (a b) -> p a b", b=128)
    rstd = A.f32(TH)
    NWB = 3
    wbuf = [A.bf16(KC * 128).rearrange("p (a b) -> p a b", b=128) for _ in range(NWB)]
    xstg = [A.f32(512) for _ in range(2)]
    markX = A.top
    XW = KC * TH
    assert markX + XW <= A.words

    wsrcs = ([w_u_d[g] for g in range(8)] + [w_q_d[c] for c in range(8)] + [w_k_d, w_vv_d]
             + [w_o_d[dc] for dc in range(16)])
    wstate = {"issued": 0, "used": 0}

    def get_w(limit=None):
        lim = len(wsrcs) if limit is None else limit
        while wstate["issued"] < min(lim, wstate["used"] + NWB):
            n = wstate["issued"]
            i = n % NWB
            dma("pool", f"wb{i}", wbuf[i], wsrcs[n], [], [f"wbuf{i}"])
            wstate["issued"] += 1
        i = wstate["used"] % NWB
        wstate["used"] += 1
        return wbuf[i], f"wbuf{i}"

    rot = {}

    def nextpb(lo=5, hi=8):
        i = rot.get((lo, hi), lo)
        rot[(lo, hi)] = lo + (i + 1 - lo) % (hi - lo)
        return i

    dma("sp", "c0", amask, amask_d, [], ["amask"])
    dma("sp", "c0", sink_bc, sink_bc_d, [], ["sink_bc"])
    dma("sp", "c0", ln_gb, ln_d, [], ["ln_gb"])

    A.top = markX
    xT = A.f32(KC * TH).rearrange("p (a b) -> p a b", b=TH)
    for i in range(4):
        dma("sp" if i % 2 == 0 else "act", "x", xT[:, 4 * i:4 * i + 4, :], xT_d[:, 4 * i:4 * i + 4, :], [], [f"xT{k}" for k in range(4 * i, 4 * i + 4)])
    w_vh_hi = A.t[:, markX + XW:markX + XW + 4096].bitcast(BF16).rearrange("p (a b) -> p a b", b=512)
    for q4 in range(4):
        dma("pool", "wv", w_vh_hi[:, 4 * q4:4 * q4 + 4, :], w_v_d[:, 4 * q4:4 * q4 + 4, 0:512], [], ["w_vh0"])
    CB = [(0, 512), (512, 512), (1024, 128)]
    for kc in range(KC):
        act(hT[:, kc, :], xT[:, kc, :], AF.Square, [f"xT{kc}"], [f"hT{kc}"])
        for bi, (c0, cn) in enumerate(CB):
            mm(pb[bi][:, 0:cn], ones_b, hT[:, kc, c0:c0 + cn], kc == 0, kc == KC - 1, ["ones_b", f"hT{kc}"], [PB[bi]])
    for bi, (c0, cn) in enumerate(CB):
        ts("dve", rstd[:, c0:c0 + cn], pb[bi][:, 0:cn], 1.0 / D, ALU.mult, [PB[bi]], ["rstd"], s2=EPS, op1=ALU.add)
    act(rstd, rstd, AF.Sqrt, ["rstd"], ["rstd"])
    S.op("dve", lambda E: E.reciprocal(out=rstd, in_=rstd), ["rstd"], ["rstd"])
    for kc in range(KC):
        stt("dve", hT[:, kc, :], xT[:, kc, :], gvec[:, 0, kc:kc + 1], rstd, ALU.mult, ALU.mult,
            [f"xT{kc}", "gvec", "rstd"], [f"hT{kc}"])
    S.barrier()
    if stage == 11:
        A.top = markX + XW
        return dbg_exit(lambda a: hT[:, a, 128:TH], 16, [f"hT{k}" for k in range(KC)])

    A.top = markX
    uT = A.bf16(8 * T).rearrange("p (a b) -> p a b", b=T)
    vn_all = A.bf16(8 * 1024).rearrange("p (a g c) -> p a g c", g=8, c=128)
    w_vh = A.bf16(KC * 512).rearrange("p (a b) -> p a b", b=512)
    vg = [A.f32(512).rearrange("p (a b) -> p a b", b=128) for _ in range(2)]
    cen = [A.f32(512).rearrange("p (a b) -> p a b", b=128) for _ in range(2)]
    sqv = A.f32(512).rearrange("p (a b) -> p a b", b=128)
    st8 = [A.f32(4 * 4).rearrange("p (a b) -> p a b", b=4) for _ in range(2)]
    sgt = [A.f32(512).rearrange("p (a b) -> p a b", b=128) for _ in range(2)]
    wsf = A.f32(1024).rearrange("p (a b) -> p a b", b=128)
    bs_bc = A.f32(1024).rearrange("p (a b) -> p a b", b=128)
    assert A.top <= markX + XW, A.top - markX
    dma("sp", "c0", wsf, wsT_d, [], ["wsf"])
    dma("sp", "c0", bs_bc, bs_bc_d, [], ["bs_bc"])
    tt("dve", wsb, wsf, cmask[:, None, :].broadcast_to([128, 8, 128]), ALU.mult, ["wsf", "cst"], ["wsb"])
    for hb in range(2):
        mm(pb[3 + hb][:, :], ones_b, wsb[:, 4 * hb:4 * hb + 4, :].rearrange("p a b -> p (a b)"), True, True,
           ["ones_b", "wsb"], [PB[3 + hb]])
    for g in range(8):
        stt("dve", Cg[:, g, :], pb[3 + g // 4][:, (g % 4) * 128:(g % 4 + 1) * 128], ln_gb[:, 1, g:g + 1], bs_bc[:, g, :],
            ALU.mult, ALU.add, [PB[3 + g // 4], "ln_gb", "bs_bc"], ["Cg"])
    for q4 in range(4):
        dma("pool", "wv", w_vh[:, 4 * q4:4 * q4 + 4, :], w_v_d[:, 4 * q4:4 * q4 + 4, 512:1024], [], ["w_vh1"])
    for hb in range(2):
        w_vb = w_vh_hi if hb == 0 else w_vh
        for tl in range(8):
            b = nextpb()
            i2 = tl % 2
            for kc in range(KC):
                mm(pb[b][:, :], hT[:, kc, 128 + tl * 128:128 + (tl + 1) * 128], w_vb[:, kc, :],
                   kc == 0, kc == KC - 1, [f"w_vh{hb}", f"hT{kc}"], [PB[b]])
            act(vg[i2].rearrange("p a b -> p (a b)"), pb[b][:, :], AF.Gelu, [PB[b]], [f"vg{i2}"])
            s8 = st8[i2]
            k8 = f"st8{i2}"
            red("dve", s8[:, 0, :], vg[i2], ALU.add, [f"vg{i2}"], [k8])
            ts("dve", s8[:, 1, :], s8[:, 0, :], -1.0 / 128, ALU.mult, [k8], [k8])
            tt("dve", cen[i2], vg[i2], s8[:, 1, :, None].broadcast_to([128, 4, 128]), ALU.add, [f"vg{i2}", k8], [f"cen{i2}"])
            tt("dve", sqv, cen[i2], cen[i2], ALU.mult, [f"cen{i2}"], ["sqv"])
            red("dve", s8[:, 2, :], sqv, ALU.add, ["sqv"], [k8])
            ts("dve", s8[:, 2, :], s8[:, 2, :], 1.0 / 128, ALU.mult, [k8], [k8], s2=EPS, op1=ALU.add)
            act(s8[:, 3, :], s8[:, 2, :], AF.Sqrt, [k8], [k8])
            S.op("dve", lambda E, s8=s8: E.reciprocal(out=s8[:, 3, :], in_=s8[:, 3, :]), [k8], [k8])
            tt("dve", vn_all[:, tl, 4 * hb:4 * hb + 4, :], cen[i2], s8[:, 3, :, None].broadcast_to([128, 4, 128]), ALU.mult,
               [f"cen{i2}", k8], [f"vn{tl}"])
    for g in range(8):
        wb, wk = get_w()
        for th in range(2):
            b = nextpb()
            for kc in range(KC):
                mm(pb[b][:, :], wb[:, kc, :], hT[:, kc, 128 + th * 512:128 + (th + 1) * 512], kc == 0, kc == KC - 1,
                   [wk, f"hT{kc}"], [PB[b]])
            act(uT[:, g, th * 512:(th + 1) * 512], pb[b][:, :], AF.Gelu, [PB[b]], ["uT"])
    for tl in range(8):
        for hb in range(2):
            b = nextpb()
            for g4 in range(4):
                g = hb * 4 + g4
                mm(pb[b][:, g4 * 128:(g4 + 1) * 128], vn_all[:, tl, g, :], wsb[:, g, :], True, True,
                   [f"vn{tl}", "wsb"], [PB[b]])
            sk = f"sgt{hb}"
            for g4 in range(4):
                g = hb * 4 + g4
                stt("dve", sgt[hb][:, g4, :], pb[b][:, g4 * 128:(g4 + 1) * 128], ln_gb[:, 0, g:g + 1], Cg[:, g, :],
                    ALU.mult, ALU.add, [PB[b], "ln_gb", "Cg"], [sk])
            tt("dve", catT[:, 4 * hb:4 * hb + 4, tl * 128:(tl + 1) * 128], sgt[hb], uT[:, 4 * hb:4 * hb + 4, tl * 128:(tl + 1) * 128],
               ALU.mult, [sk, "uT"], [f"cat{tl}"])
    S.barrier()
    if stage == 12:
        A.top = markX + XW
        return dbg_exit(lambda a: catT[:, a, :], 8, [f"cat{t}" for t in range(8)])

    A.top = markX
    qT = A.bf16(8 * T).rearrange("p (a b) -> p a b", b=T)
    kT = A.bf16(TH)
    vtok = A.bf16(9 * 128).rearrange("p (a b) -> p a b", b=128)
    kbd = A.bf16(8 * 512).rearrange("p (a b) -> p a b", b=512)
    sc = [A.f32(2048).rearrange("p (a b) -> p a b", b=256) for _ in range(3)]
    pn = [A.bf16(2048).rearrange("p (a b) -> p a b", b=256) for _ in range(2)]
    pT = [A.bf16(2048).rearrange("p (a b) -> p a b", b=128) for _ in range(2)]
    sm = [A.f32(64).rearrange("p (a b) -> p a b", b=8) for _ in range(3)]
    assert A.top <= markX + XW
    for c in range(8):
        wb, wk = get_w()
        for th in range(2):
            b = nextpb()
            for kc in range(KC):
                mm(pb[b][:, :], wb[:, kc, :], hT[:, kc, 128 + th * 512:128 + (th + 1) * 512], kc == 0, kc == KC - 1,
                   [wk, f"hT{kc}"], [PB[b]])
            ts("dve", qT[:, c, th * 512:(th + 1) * 512], pb[b][:, :], 0.125, ALU.mult, [PB[b]], ["qT"])
    wb, wk = get_w()
    for bi, (c0, cn) in enumerate(CB):
        b = nextpb()
        for kc in range(KC):
            mm(pb[b][:, 0:cn], wb[:, kc, :], hT[:, kc, c0:c0 + cn], kc == 0, kc == KC - 1, [wk, f"hT{kc}"], [PB[b]])
        cp("act", kT[:, c0:c0 + cn], pb[b][:, 0:cn], [PB[b]], ["kT"])
    wb, wk = get_w()
    for tl in range(9):
        b = nextpb()
        for kc in range(KC):
            mm(pb[b][:, 0:128], hT[:, kc, tl * 128:(tl + 1) * 128], wb[:, kc, :], kc == 0, kc == KC - 1,
               [wk, f"hT{kc}"], [PB[b]])
        cp("act", vtok[:, tl, :], pb[b][:, 0:128], [PB[b]], ["vtok"])
    S.op("dve", lambda E: E.memset(kbd, 0.0), [], ["kbd"])
    for blk in range(8):
        cp("dve", kbd[0:64, blk, 0:256], kT[0:64, blk * 128:blk * 128 + 256], ["kT"], ["kbd"])
        cp("act", kbd[64:128, blk, 256:512], kT[64:128, blk * 128:blk * 128 + 256], ["kT"], ["kbd"])
    if stage == 131:
        S.barrier()
        A.top = markX + XW
        return dbg_exit(lambda a: qT[:, a, :], 8, ["qT"])
    sink_v = sink_bc.rearrange("p (s c) -> p c s", s=2)
    iters = [(blk, c4) for blk in range(8) for c4 in range(2)]

    def att_s1a(n):
        blk, c4 = iters[n]
        i3 = n % 3
        mb = 0 if blk == 0 else 1
        for c_ in range(4):
            mm(pb[c_], qT[:, c4 * 4 + c_, blk * 128:(blk + 1) * 128], kbd[:, blk, :], True, True, ["qT", "kbd"], [PB[c_]])
        m_ = sm[i3]
        mk = f"sm{i3}"
        snk = sink_v[:, c4 * 4:c4 * 4 + 4, :]
        tt("dve", sc[i3], psA[:, 0:2048].rearrange("p (a b) -> p a b", b=256), amask[:, mb:mb + 1, :].broadcast_to([128, 8, 256]),
           ALU.add, PB[0:4] + ["amask"], [f"sc{i3}"])
        red("dve", m_[:, 0, :], sc[i3], ALU.max, [f"sc{i3}"], [mk])
        tt("dve", m_[:, 1, :].rearrange("p (c s) -> p c s", s=2), m_[:, 0, :].rearrange("p (c s) -> p c s", s=2), snk, ALU.max,
           [mk, "sink_bc"], [mk])
        ts("dve", m_[:, 2, :], m_[:, 1, :], -1.0, ALU.mult, [mk], [mk])
        tt("dve", m_[:, 4, :].rearrange("p (c s) -> p c s", s=2), snk, m_[:, 1, :].rearrange("p (c s) -> p c s", s=2), ALU.subtract,
           [mk, "sink_bc"], [mk])

    def att_s1b(n):
        blk, c4 = iters[n]
        i3 = n % 3
        i2 = n % 2
        m_ = sm[i3]
        mk = f"sm{i3}"
        for h in range(8):
            act(sc[i3][:, h, :], sc[i3][:, h, :], AF.Exp, [f"sc{i3}", mk], [f"sc{i3}", mk],
                bias=m_[:, 2, h:h + 1], scale=1.0, accum_out=m_[:, 3, h:h + 1])
        act(m_[:, 5, :], m_[:, 4, :], AF.Exp, [mk], [mk])
        tt("dve", m_[:, 6, :], m_[:, 5, :], m_[:, 3, :], ALU.add, [mk], [mk])
        S.op("dve", lambda E, m_=m_: E.reciprocal(out=m_[:, 7, :], in_=m_[:, 6, :]), [mk], [mk])
        tt("pool", pn[i2], sc[i3], m_[:, 7, :, None].broadcast_to([128, 8, 256]), ALU.mult, [f"sc{i3}", mk], [f"pn{i2}"])

    def att_s2(n):
        blk, c4 = iters[n]
        i2 = n % 2
        ptv = psA[:, 2048:3072].bitcast(BF16).rearrange("p (a b) -> p a b", b=128)
        for h in range(8):
            for kc2 in range(2):
                tr(ptv[:, h * 2 + kc2, :], pn[i2][:, h, kc2 * 128:(kc2 + 1) * 128], ident_b, [f"pn{i2}", "ident_b"], [PB[4 + h // 4]])
        cp("act", pT[i2], ptv, [PB[4], PB[5]], [f"pT{i2}"])
        for c_ in range(4):
            for s_ in range(2):
                lo, hi = s_ * 64, (s_ + 1) * 64
                for kc2 in range(2):
                    mm(pb[6][lo:hi, c_ * 128:(c_ + 1) * 128], vtok[:, blk + kc2, lo:hi], pT[i2][:, (c_ * 2 + s_) * 2 + kc2, :],
                       kc2 == 0, kc2 == 1, ["vtok", f"pT{i2}"], [PB[6]])
        cp("dve", catT[:, 8 + c4 * 4:8 + c4 * 4 + 4, blk * 128:(blk + 1) * 128], pb[6].rearrange("p (a b) -> p a b", b=128),
           [PB[6]], [f"cat{blk}"])

    NI = len(iters)
    att_s1a(0)
    att_s1a(1)
    att_s1b(0)
    for n in range(NI):
        if n + 2 < NI:
            att_s1a(n + 2)
        if n + 1 < NI:
            att_s1b(n + 1)
        att_s2(n)
    S.barrier()
    if stage == 13:
        A.top = markX + XW
        return dbg_exit(lambda a: catT[:, a, :], 16, [f"cat{t}" for t in range(8)])

    A.top = markX
    x1T = A.f32(KC * T).rearrange("p (a b) -> p a b", b=T)
    H2P_OFF = mark0 + 40256 + 5920 + 1024
    assert H2P_OFF >= markX + XW and H2P_OFF + 4096 <= A.words
    h2p0 = A.t[:, H2P_OFF:H2P_OFF + 4096].bitcast(BF16).rearrange("p (a b) -> p a b", b=512)
    CAT = [f"cat{t}" for t in range(8)]
    HT = [f"hT{k}" for k in range(KC)]

    def norm2_acc(kc):
        act(hT[:, kc, 0:T], x1T[:, kc, :], AF.Square, [f"x1T{kc}"], [f"hT{kc}"])
        for th in range(2):
            mm(pb[th][:, :], ones_b, hT[:, kc, th * 512:(th + 1) * 512], kc == 0, kc == KC - 1, ["ones_b", f"hT{kc}"], [PB[th]])
        dma("sp", "sp1", x1s_d[:, kc, :], x1T[:, kc, :], [f"x1T{kc}"], ["x1s"])
    xi = 0
    for dc in range(16):
        wb, wk = get_w()
        for th in range(2):
            b = nextpb()
            xs_ = xstg[xi % 2]
            xk = f"xstg{xi % 2}"
            xi += 1
            dma("sp", xk, xs_, xT_d[:, dc, 128 + th * 512:128 + (th + 1) * 512], [], [xk])
            for fc in range(KC):
                mm(pb[b][:, :], wb[:, fc, :], catT[:, fc, th * 512:(th + 1) * 512], fc == 0, fc == KC - 1,
                   [wk] + CAT[4 * th:4 * th + 4], [PB[b]])
            tt("dve", x1T[:, dc, th * 512:(th + 1) * 512], xs_, pb[b][:, :], ALU.add, [PB[b], xk], [f"x1T{dc}"])
        if dc > 0:
            norm2_acc(dc - 1)
    norm2_acc(KC - 1)
    for th in range(2):
        ts("dve", rstd[:, th * 512:(th + 1) * 512], pb[th][:, :], 1.0 / D, ALU.mult, [PB[th]], ["rstd"], s2=EPS, op1=ALU.add)
    act(rstd[:, 0:T], rstd[:, 0:T], AF.Sqrt, ["rstd"], ["rstd"])
    S.op("dve", lambda E: E.reciprocal(out=rstd[:, 0:T], in_=rstd[:, 0:T]), ["rstd"], ["rstd"])
    for kc in range(KC):
        stt("dve", h2p0[:, kc, :], x1T[:, kc, 0:512], gvec[:, 1, kc:kc + 1], rstd[:, 0:512], ALU.mult, ALU.mult,
            [f"x1T{kc}", "gvec", "rstd"], ["h2p"])
    for kc in range(KC):
        stt("dve", hT[:, kc, 512:T], x1T[:, kc, 512:T], gvec[:, 1, kc:kc + 1], rstd[:, 512:T], ALU.mult, ALU.mult,
            [f"x1T{kc}", "gvec", "rstd"], [f"hT{kc}"])
        if kc % 4 == 3:
            dma("sp", "sp2", h2s_d[:, kc - 3:kc + 1, :], hT[:, kc - 3:kc + 1, 512:T], HT[kc - 3:kc + 1], ["h2s"])
    S.barrier()

    TP = 512
    A.top = mark0
    Wb = A.bf16(128 * TP).rearrange("p (j t) -> p j t", t=TP)
    rank = A.f32(4 * TP).rearrange("p (a t) -> p a t", t=TP)
    q2T = A.bf16(8 * TP).rearrange("p (h t) -> p h t", t=TP)
    IfTb = A.bf16(TP)
    iota_b = A.bf16(128)
    cp("dve", iota_b, iota_f, ["cst"], ["iota_b"])
    wb2_off = A.top
    wbuf2 = [A.bf16(KC * 128).rearrange("p (a b) -> p a b", b=128) for _ in range(NWB)]
    WB2K = [f"wbuf2_{i}" for i in range(NWB)]
    markZ = A.top
    NWQ = NWB + 1
    WQ3_OFF = mark0 + 40256 + 5920 + 1024 + 4096
    assert WQ3_OFF + 1024 <= A.words
    wbuf2.append(A.t[:, WQ3_OFF:WQ3_OFF + 1024].bitcast(BF16).rearrange("p (a b) -> p a b", b=128))
    wq_srcs = [w_qr_d[cc] for cc in range(16)] * 2
    wq_state = {"issued": 0, "used": 0}

    def get_wq():
        lim = (wq_state["used"] // 16 + 1) * 16
        while wq_state["issued"] < min(lim, wq_state["used"] + NWQ):
            n = wq_state["issued"]
            i = n % NWQ
            dma("pool", f"wq{i}", wbuf2[i], wq_srcs[n], [], [f"wbuf2_{i}"])
            wq_state["issued"] += 1
        i = wq_state["used"] % NWQ
        wq_state["used"] += 1
        return wbuf2[i], f"wbuf2_{i}"

    x2T_e = A.t[:, mark0:mark0 + KC * T].rearrange("p (a b) -> p a b", b=T)
    for tp in range(2):
        tok0 = tp * TP
        A.top = markZ
        ND = 4
        dwb = [A.bf16(KC * 128).rearrange("p (a b) -> p a b", b=128) for _ in range(ND)]
        q1T = A.bf16(8 * TP).rearrange("p (h t) -> p h t", t=TP)
        cand = A.f32(256).rearrange("p (r c) -> p r c", c=16)
        sm8 = A.f32(32).rearrange("p (a h) -> p a h", h=8)
        rk = A.f32(512).rearrange("p (a h r) -> p a h r", h=8, r=16)
        assert A.top == H2P_OFF, (A.top, H2P_OFF)
        h2p = A.bf16(KC * TP).rearrange("p (a b) -> p a b", b=TP)
        scr = A.t[:, wb2_off:wb2_off + 3072]
        o_ = {"n": 0}

        def sf32(n):
            off = o_["n"]
            o_["n"] += (n + 7) // 8 * 8
            assert o_["n"] <= 3072
            return scr[:, off:off + n]
        s12 = sf32(2048).rearrange("p (a b) -> p a b", b=128)
        wk1 = sf32(128)
        wk2 = sf32(256)
        v1 = sf32(128).rearrange("p (h r) -> p h r", r=16)
        v2 = sf32(128).rearrange("p (h r) -> p h r", r=16)
        I1 = sf32(128).bitcast(U32).rearrange("p (h r) -> p h r", r=16)
        tv = sf32(128).rearrange("p (h r) -> p h r", r=16)
        ev = sf32(128).rearrange("p (h r) -> p h r", r=16)

        dst = {"issued": 0}

        def get_d(j):
            while dst["issued"] < min(128, j + ND):
                n = dst["issued"]
                dma("pool", f"dw{n % ND}", dwb[n % ND], dwn_d[n], [], [f"dwb{n % ND}"])
                dst["issued"] += 1
            return dwb[j % ND], f"dwb{j % ND}"

        for cc in range(16):
            wb, wk = get_wq()
            b = nextpb(4, 8)
            for kc in range(KC):
                mm(pb[b][:, :], wb[:, kc, :], h2p[:, kc, :], kc == 0, kc == KC - 1, [wk, "h2p"], [PB[b]])
            h, half = cc // 2, cc % 2
            if half == 0:
                cp("act", q1T[:, h, :], pb[b][:, :], [PB[b]], ["q1T"])
            else:
                cp("dve", q2T[:, h, :], pb[b][:, :], [PB[b]], ["q2T"])
            if cc == 11:
                get_d(0)

        def prepA(tl):
            tc0 = tl * 128
            for cc in range(16):
                h, half = cc // 2, cc % 2
                src = q1T if half == 0 else q2T
                mm(pb[4 + cc // 4][:, (cc % 4) * 128:(cc % 4 + 1) * 128], src[:, h, tc0:tc0 + 128], keysT[:, half, :], True, True,
                   ["q1T", "q2T", "keysT"], [PB[4 + cc // 4]])
            for b4 in range(4):
                cp("dve", s12[:, 4 * b4:4 * b4 + 4, :], pb[4 + b4][:, :].rearrange("p (a b) -> p a b", b=128), [PB[4 + b4]], ["s12"] + WB2K)
            for h in range(8):
                a1 = s12[:, 2 * h, :]
                a2 = s12[:, 2 * h + 1, :]
                S.op("dve", lambda E, a1=a1, h=h: E.max(out=v1[:, h, 0:8], in_=a1), ["s12"], ["v1"])
                S.op("dve", lambda E, a1=a1, h=h: E.max_index(out=I1[:, h, 0:8], in_max=v1[:, h, 0:8], in_values=a1), ["s12", "v1"], ["I1"])
                S.op("dve", lambda E, a1=a1, h=h: E.match_replace(out=wk1, in_to_replace=v1[:, h, 0:8], in_values=a1, imm_value=-1e30), ["s12", "v1"], ["wk1"])
                S.op("dve", lambda E, h=h: E.max(out=v1[:, h, 8:16], in_=wk1), ["wk1"], ["v1"])
                S.op("dve", lambda E, a1=a1, h=h: E.max_index(out=I1[:, h, 8:16], in_max=v1[:, h, 8:16], in_values=a1), ["s12", "v1"], ["I1"])
                S.op("dve", lambda E, a2=a2, h=h: E.max(out=v2[:, h, 0:8], in_=a2), ["s12"], ["v2"])
                S.op("dve", lambda E, a2=a2, h=h: E.match_replace(out=wk1, in_to_replace=v2[:, h, 0:8], in_values=a2, imm_value=-1e30), ["s12", "v2"], ["wk1"])
                S.op("dve", lambda E, h=h: E.max(out=v2[:, h, 8:16], in_=wk1), ["wk1"], ["v2"])
            ch = cand.rearrange("p r c -> p (r c)")
            for h in range(8):
                tt("dve", cand, v1[:, h, :, None].broadcast_to([128, 16, 16]), v2[:, h, None, :].broadcast_to([128, 16, 16]), ALU.add,
                   ["v1", "v2"], ["cand"])
                S.op("dve", lambda E, h=h: E.max(out=tv[:, h, 0:8], in_=ch), ["cand"], ["tv"])
                S.op("dve", lambda E, h=h: E.match_replace(out=wk2, in_to_replace=tv[:, h, 0:8], in_values=ch, imm_value=-1e30), ["cand", "tv"], ["wk2"])
                S.op("dve", lambda E, h=h: E.max(out=tv[:, h, 8:16], in_=wk2), ["wk2"], ["tv"])
            tt("dve", ev, tv, tv[:, :, 0:1].broadcast_to([128, 8, 16]), ALU.subtract, ["tv"], ["ev"])

        def prepB(tl):
            act(ev, ev, AF.Exp, ["ev"], ["ev"])
            red("dve", sm8[:, 0, :], ev, ALU.add, ["ev"], ["sm8"])
            ts("dve", sm8[:, 2, :], tv[:, :, 15], -1e-4, ALU.add, ["tv"], ["sm8"])
            tt("dve", rk[:, 0, :, :], sm8[:, 2, :, None].broadcast_to([128, 8, 16]), v1, ALU.subtract, ["sm8", "v1"], ["rk"])
            act(sm8[:, 3, :], sm8[:, 0, :], AF.Ln, ["sm8"], ["sm8"])
            tt("dve", sm8[:, 3, :], sm8[:, 3, :], v2[:, :, 0], ALU.add, ["sm8", "v2"], ["sm8"])
            tt("dve", sm8[:, 3, :], sm8[:, 3, :], v1[:, :, 0], ALU.add, ["sm8", "v1"], ["sm8"])
            tt("dve", rk[:, 3, :, :], v1, sm8[:, 3, :, None].broadcast_to([128, 8, 16]), ALU.subtract, ["v1", "sm8"], ["rk"])
            cp("dve", rk[:, 2, :, :], I1, ["I1"], ["rk"])
            cp("dve", rk[:, 1, :, :], I1, ["I1"], ["rk"])

        def prepC(tl):
            tc0 = tl * 128
            bT = 4 + tl % 2
            for a in range(4):
                tr(pb[bT][:, a * 128:(a + 1) * 128], rk[:, a, :, :].rearrange("p h r -> p (h r)"), ident_f, ["rk", "cst"], [PB[bT]])
            cp("dve", rank[:, :, tc0:tc0 + 128], pb[bT][:, :].rearrange("p (a b) -> p a b", b=128), [PB[bT]], ["rank"])
            cp("dve", IfTb[:, tc0:tc0 + 128], pb[bT][:, 256:384], [PB[bT]], ["rank"])

        hooks = {}
        for tl in range(4):
            j0 = 2 + 31 * tl
            hooks[j0] = (prepA, tl)
            hooks[j0 + 16] = (prepB, tl)
            hooks[j0 + 24] = (prepC, tl)

        for j in range(128):
            dw, dk = get_d(j)
            b = nextpb(0, 4)
            for kc in range(KC):
                mm(pb[b][:, :], dw[:, kc, :], h2p[:, kc, :], kc == 0, kc == KC - 1, [dk, "h2p"], [PB[b]])
            act(Wb[:, j, :], pb[b][:, :], AF.Gelu, [PB[b]], [f"W{j // 16}"])
            if j in hooks:
                fn_, tl_ = hooks[j]
                fn_(tl_)
        S.barrier()
        if stage == 21:
            S.finish()
            es.close()
            return nc

        A.top = markZ
        NB = 8
        q2rep = [A.bf16(NB * 128).rearrange("p (t m) -> p t m", m=128) for _ in range(2)]
        Lb = [A.bf16(NB * 128).rearrange("p (t m) -> p t m", m=128) for _ in range(2)]
        Rb = [A.bf16(NB * 128).rearrange("p (t m) -> p t m", m=128) for _ in range(2)]
        ebb = [A.bf16(NB * 128).rearrange("p (t m) -> p t m", m=128) for _ in range(2)]
        mskb = [A.bf16(NB * 128).rearrange("p (t m) -> p t m", m=128) for _ in range(2)]
        WK = [f"W{k}" for k in range(8)]
        nbat = TP // NB

        def pAv(i2):
            return psA[:, i2 * 1024:(i2 + 1) * 1024].rearrange("p (t m) -> p t m", m=128), [PB[2 * i2], PB[2 * i2 + 1]]

        def pGv(i2):
            return psA[:, 2048 + i2 * 1024:2048 + (i2 + 1) * 1024].rearrange("p (t m) -> p t m", m=128), [PB[4 + 2 * i2], PB[5 + 2 * i2]]

        def frontA1(tb):
            t0 = tb * NB
            i2 = tb % 2
            pA, kA = pAv(i2)
            cp("act", q2rep[i2].rearrange("p t (h r) -> p t h r", r=16),
               q2T[:, :, t0:t0 + NB].rearrange("p h t -> p t h")[:, :, :, None].broadcast_to([128, NB, 8, 16]), ["q2T"], [f"q2rep{i2}"])

        def frontA1mm(tb):
            i2 = tb % 2
            pA, kA = pAv(i2)
            for k in range(NB):
                mm(pA[:, k, :], q2rep[i2][:, k, :], keysT[:, 1, :], True, True, [f"q2rep{i2}", "keysT"], [kA[k // 4]])

        def frontA2(tb):
            t0 = tb * NB
            i2 = tb % 2
            pA, kA = pAv(i2)
            H = NB // 2

            def exps(hf):
                for k in range(hf * H, (hf + 1) * H):
                    act(ebb[i2][:, k, :], pA[:, k, :], AF.Exp, ["rank"], [f"eb{i2}{hf}", kA[hf]], bias=rank[:, 3, t0 + k:t0 + k + 1], scale=1.0)

            def mask(hf):
                tt("dve", mskb[i2][:, hf * H:(hf + 1) * H, :], pA[:, hf * H:(hf + 1) * H, :],
                   rank[:, 0, t0 + hf * H:t0 + (hf + 1) * H, None].broadcast_to([128, H, 128]), ALU.is_ge, ["rank"], [f"msk{i2}{hf}", kA[hf]])
            mask(0)
            exps(1)
            tt("dve", Lb[i2], iota_b[:, None, :].broadcast_to([128, NB, 128]), IfTb[:, t0:t0 + NB, None].broadcast_to([128, NB, 128]),
               ALU.is_equal, ["iota_b", "rank"], [f"L{i2}"])
            mask(1)
            exps(0)
            for hf in (1, 0):
                tt("pool", Rb[i2][:, hf * H:(hf + 1) * H, :], mskb[i2][:, hf * H:(hf + 1) * H, :], ebb[i2][:, hf * H:(hf + 1) * H, :], ALU.mult,
                   [f"msk{i2}{hf}", f"eb{i2}{hf}"], [f"R{i2}{hf}"])

        def backGmm(tb):
            i2 = tb % 2
            pG, kG = pGv(i2)
            H = NB // 2
            for k in list(range(H, NB)) + list(range(0, H)):
                mm(pG[:, k, :], Lb[i2][:, k, :], Rb[i2][:, k, :], True, True, [f"L{i2}", f"R{i2}{k // H}"], [kG[k // 4]])

        def backW(tb):
            t0 = tb * NB
            i2 = tb % 2
            pG, kG = pGv(i2)
            wv = Wb[:, :, t0:t0 + NB]
            tt("dve", wv, pG.rearrange("p t j -> p j t"), wv, ALU.mult, kG + WK, WK)

        frontA1(0)
        frontA1mm(0)
        if nbat > 1:
            frontA1(1)
            frontA1mm(1)
        frontA2(0)
        for tb in range(nbat):
            if tb + 2 < nbat:
                frontA1(tb + 2)
            backGmm(tb)
            if tb + 2 < nbat:
                frontA1mm(tb + 2)
            if tb + 1 < nbat:
                frontA2(tb + 1)
            backW(tb)
        S.barrier()
        if stage == 22:
            S.finish()
            es.close()
            return nc

        A.top = wb2_off
        NUB = 5
        xs3 = [A.f32(512) for _ in range(8)]
        upb = [A.bf16(2 * 1024).rearrange("p (a b) -> p a b", b=1024) for _ in range(NUB)]
        assert A.top <= H2P_OFF
        if tp == 0:
            dma("sp", "h2p", h2p, h2s_d, ["h2s"], ["h2p"])
        for dh in range(2):
            ust = {"issued": 0}

            def get_u(jp, dh=dh, ust=ust):
                while ust["issued"] < min(64, jp + NUB):
                    n = ust["issued"]
                    dma("pool", f"up{n % NUB}", upb[n % NUB], upw_d[dh, 2 * n:2 * n + 2].rearrange("a i n -> i a n"), [], [f"upb{n % NUB}"])
                    ust["issued"] += 1
                return upb[jp % NUB], f"upb{jp % NUB}"
            for dc in range(8):
                dma("sp", "xs3l", xs3[dc], x1s_d[:, dh * 8 + dc, tok0:tok0 + TP], ["x1s"], [f"xs3{dc}"])
            last = (tp == 1 and dh == 1)
            for j in range(128):
                ub, uk = get_u(j // 2)
                for dc in range(8):
                    mm(pb[dc], ub[:, j % 2, dc * 128:(dc + 1) * 128], Wb[:, j, :], j == 0, j == 127, [uk, f"W{j // 16}"], [PB[dc]])
                if last and j in (64, 80, 96, 108):
                    WD = ["W0", "W1", "W2", "W3"]
                    i = (64, 80, 96, 108).index(j)
                    tsl = slice(0, T) if i < 2 else slice(0, TP)
                    dma("act", "x2l", x2T_e[:, 4 * i:4 * i + 4, tsl], x1s_d[:, 4 * i:4 * i + 4, tsl], ["x2s", "x1s"],
                        [f"x2T{k}" for k in range(4 * i, 4 * i + 4)] + WD)
            for dc in range(8):
                kc = dh * 8 + dc
                if last:
                    tt("dve", x2T_e[:, kc, tok0:tok0 + TP], xs3[dc], pb[dc], ALU.add, [PB[dc], f"xs3{dc}"], [f"x2T{kc}"])
                else:
                    tt("dve", xs3[dc], xs3[dc], pb[dc], ALU.add, [PB[dc], f"xs3{dc}"], [f"xs3{dc}"])
                    dma("sp", "xo3", x1s_d[:, kc, tok0:tok0 + TP], xs3[dc], [f"xs3{dc}"], ["x2s"])
        S.barrier()

    A.top = mark0
    x2T = A.f32(KC * T).rearrange("p (a b) -> p a b", b=T)
    sq2 = A.bf16(KC * T).rearrange("p (a b) -> p a b", b=T)
    rs3 = A.f32(T)
    assert A.top - KC * T - KC * T // 2 - T == mark0
    for kc in range(KC):
        if kc % 3 == 2:
            tt("dve", sq2[:, kc, :], x2T[:, kc, :], x2T[:, kc, :], ALU.mult, [f"x2T{kc}"], [f"sq2{kc}"])
        else:
            act(sq2[:, kc, :], x2T[:, kc, :], AF.Square, [f"x2T{kc}"], [f"sq2{kc}"])
        for th in range(2):
            mm(pb[th][:, :], ones_b, sq2[:, kc, th * 512:(th + 1) * 512], kc == 0, kc == KC - 1, ["ones_b", f"sq2{kc}"], [PB[th]])
    for th in range(2):
        ts("dve", rs3[:, th * 512:(th + 1) * 512], pb[th][:, :], 1.0 / D, ALU.mult, [PB[th]], ["rs3"], s2=EPS, op1=ALU.add)
    act(rs3, rs3, AF.Sqrt, ["rs3"], ["rs3"])
    S.op("dve", lambda E: E.reciprocal(out=rs3, in_=rs3), ["rs3"], ["rs3"])
    for kc in range(KC):
        stt("dve", x2T[:, kc, :], x2T[:, kc, :], gvec[:, 2, kc:kc + 1], rs3, ALU.mult, ALU.mult, [f"x2T{kc}", "gvec", "rs3"], [f"x2T{kc}"])
    for i in range(4):
        dma("sp" if i % 2 == 0 else "act", "y", yT_d[:, 4 * i:4 * i + 4, :], x2T[:, 4 * i:4 * i + 4, :], [f"x2T{k}" for k in range(4 * i, 4 * i + 4)], ["y"])

    S.finish()
    es.close()
    return nc


def _prep(inputs):
    f = np.float32
    x = np.asarray(inputs["x"], f)
    w_in = np.asarray(inputs["w_in"], f)[0]
    common = {}

    def chunked(w, ncol_chunks):
        return np.ascontiguousarray(w.reshape(KC, 128, ncol_chunks, 128).transpose(2, 1, 0, 3))

    common["w_u"] = chunked(w_in[:, 0:1024], 8)
    common["w_v"] = np.ascontiguousarray(w_in[:, 1024:2048].reshape(KC, 128, 1024).transpose(1, 0, 2))
    wq = w_in[:, 2048:3072].reshape(D, 16, 64)
    wq_perm = np.stack([wq[:, 0:8], wq[:, 8:16]], axis=2).reshape(D, 1024)
    common["w_q"] = chunked(wq_perm, 8)
    common["w_k"] = np.ascontiguousarray(w_in[:, 3072:3200].reshape(KC, 128, 128).transpose(1, 0, 2))
    common["w_vv"] = np.ascontiguousarray(w_in[:, 3200:3328].reshape(KC, 128, 128).transpose(1, 0, 2))
    gv = np.stack([np.asarray(inputs["norm1_g"], f)[0], np.asarray(inputs["norm2_g"], f)[0], np.asarray(inputs["norm_f_g"], f)], 0)
    common["gvec"] = np.ascontiguousarray(gv.reshape(3, KC, 128).transpose(2, 0, 1))
    common["wsT"] = np.ascontiguousarray(np.asarray(inputs["w_spatial"], f)[0].transpose(2, 0, 1))
    common["bs_bc"] = np.ascontiguousarray(np.broadcast_to(np.asarray(inputs["b_spatial"], f)[0][None], (128, 8, 128)))
    common["sink_bc"] = np.ascontiguousarray(np.broadcast_to(np.asarray(inputs["attn_sinks"], f)[0][None], (128, 16)))
    common["ln_gb"] = np.ascontiguousarray(np.stack([np.asarray(inputs["sgu_ln_g"], f)[0].T, np.asarray(inputs["sgu_ln_b"], f)[0].T], 1))
    w_out = np.asarray(inputs["w_out"], f)[0]
    wo_a = w_out[0:1024]
    wo_b = w_out[1024:2048].reshape(16, 64, D)
    wo_bp = np.stack([wo_b[0:8], wo_b[8:16]], axis=1).reshape(1024, D)
    common["w_o"] = chunked(np.concatenate([wo_a, wo_bp], 0), 16)
    common["w_qr"] = chunked(np.asarray(inputs["w_query"], f)[0], 16)
    common["keysT"] = np.ascontiguousarray(np.asarray(inputs["sub_keys"], f)[0].transpose(2, 0, 1))
    ed = np.asarray(inputs["expert_down"], f)[0]
    common["dwn"] = np.ascontiguousarray(ed.reshape(128, 128, KC, 128).transpose(1, 3, 2, 0))
    eu = np.asarray(inputs["expert_up"], f)[0]
    common["upw"] = np.ascontiguousarray(eu.reshape(128, 128, 2, 1024).transpose(2, 1, 0, 3))
    ident = np.eye(128, dtype=f)
    iota = np.broadcast_to(np.arange(128, dtype=f)[None], (128, 128))
    cm = (np.arange(128)[:, None] <= np.arange(128)[None, :]).astype(f)
    common["cst"] = np.ascontiguousarray(np.stack([ident, iota, cm], 1))
    i = np.arange(128)[:, None]
    j = np.arange(256)[None, :]
    band = (j >= i + 1) & (j <= i + 128)
    m_all = np.where(band, 0.0, NEG).astype(f)
    m_first = np.where(band & (j >= 128), 0.0, NEG).astype(f)
    in_maps = []
    for c in range(NCORES):
        b, half = c // 2, c % 2
        t0 = half * T
        xs = np.zeros((TH, D), f)
        xs[128:] = x[b, t0:t0 + T]
        if half == 1:
            xs[:128] = x[b, t0 - 128:t0]
        m = dict(common)
        m["xT"] = np.ascontiguousarray(xs.T.reshape(KC, 128, TH).transpose(1, 0, 2))
        m["amask"] = np.ascontiguousarray(np.stack([m_first if half == 0 else m_all, m_all], 1))
        in_maps.append(m)
    return in_maps


def _gather(res):
    y = np.empty((4, 2048, D), np.float32)
    for c in range(NCORES):
        b, half = c // 2, c % 2
        yT = np.asarray(res.results[c]["yT"])
        y[b, half * T:(half + 1) * T] = yT.transpose(2, 1, 0).reshape(T, D)
    return y


def kernel(**inputs):
    in_maps = _prep(inputs)
    nc = build()
    res = run_bass_kernel_spmd(nc, in_maps, core_ids=list(range(NCORES)))
    return _gather(res)
```

```python
import contextlib
import numpy as np
import concourse.bass as bass
import concourse.mybir as mybir
from concourse.bass_utils import run_bass_kernel_spmd

F32 = mybir.dt.float32
BF16 = mybir.dt.bfloat16
U32 = mybir.dt.uint32
AF = mybir.ActivationFunctionType
ALU = mybir.AluOpType
AX = mybir.AxisListType

NCORES = 8
T = 1024
TH = 1152
D = 2048
KC = 16
EPS = 1e-6
NEG = -30000.0


_NEED = None


class Sched:
    ENGS = ("pe", "act", "dve", "pool", "sp")
    last_need = None

    def __init__(self, nc):
        import bisect
        self._bisect = bisect
        self.nc = nc
        self.dry = _NEED is None
        self.need = {e: set() for e in ("pe", "act", "dve", "pool")} if self.dry else None
        self.rank = None if self.dry else {e: sorted(v) for e, v in _NEED.items()}
        self.needset = None if self.dry else _NEED
        self.streams = {e: [] for e in self.ENGS}
        self.cnt = {}
        self.semh = {}
        self.waited = {e: {} for e in self.ENGS}
        self.last_w = {}
        self.readers = {}
        self._ctx = []
        for e in ("pe", "act", "dve", "pool"):
            self._mk_sem(e)

    def _mk_sem(self, name):
        cm = self.nc.semaphore("s_" + name)
        h = cm.__enter__()
        self._ctx.append(cm)
        self.semh[name] = h
        self.cnt[name] = 0

    def _val(self, dom, c):
        if dom.startswith("d_"):
            return c
        if self.dry:
            self.need[dom].add(c)
            return c
        return self._bisect.bisect_right(self.rank[dom], c)

    def _deps(self, eng, reads, writes):
        deps = {}

        def add(dom, c):
            if dom.startswith("d_"):
                c = self.cnt[dom]
            deps[dom] = max(deps.get(dom, 0), c)
        for k in reads:
            lw = self.last_w.get(k)
            if lw:
                add(*lw)
        for k in writes:
            lw = self.last_w.get(k)
            if lw and lw[0] != eng:
                add(*lw)
            for dom, c in self.readers.get(k, {}).items():
                if dom != eng:
                    add(dom, c)
        out = []
        for dom, c in deps.items():
            if self.waited[eng].get(dom, 0) < c:
                self.waited[eng][dom] = c
                out.append((dom, self._val(dom, c)))
        return out

    def _record(self, dom, my, reads, writes):
        for k in reads:
            self.readers.setdefault(k, {})[dom] = my
        for k in writes:
            self.last_w[k] = (dom, my)
            self.readers[k] = {}

    def op(self, eng, fn, reads=(), writes=()):
        waits = self._deps(eng, reads, writes)
        self.cnt[eng] += 1
        my = self.cnt[eng]
        sem = self.semh[eng]
        semh = self.semh
        sig = self.dry or (my in self.needset[eng])

        def thunk(E):
            for dom, v in waits:
                E.wait_ge(semh[dom], v)
            ins = fn(E)
            if sig:
                ins.then_inc(sem, 1)
        self.streams[eng].append(thunk)
        self._record(eng, my, reads, writes)

    def dma(self, q, dsem, fn, reads=(), writes=()):
        dom = "d_" + dsem
        if dom not in self.semh:
            self._mk_sem(dom)
        waits = self._deps(q, reads, writes)
        self.cnt[dom] += 16
        my = self.cnt[dom]
        sem = self.semh[dom]
        semh = self.semh

        def thunk(E):
            for d, v in waits:
                E.wait_ge(semh[d], v)
            fn(E).then_inc(sem, 16)
        self.streams[q].append(thunk)
        self._record(dom, my, reads, writes)

    def barrier(self):
        snap = dict(self.cnt)
        semh = self.semh
        for e in self.ENGS:
            waits = []
            for dom, c in snap.items():
                if c > 0 and self.waited[e].get(dom, 0) < c:
                    self.waited[e][dom] = c
                    waits.append((dom, self._val(dom, c)))

            def thunk(E, waits=waits):
                for d, v in waits:
                    E.wait_ge(semh[d], v)
            self.streams[e].append(thunk)

    def finish(self):
        self.barrier()
        if self.dry:
            Sched.last_need = self.need
            for cm in reversed(self._ctx):
                cm.__exit__(None, None, None)
            return
        nc = self.nc
        streams = self.streams
        with nc.Block() as block:
            @block.tensor
            def _(E):
                for t in streams["pe"]:
                    t(E)

            @block.scalar
            def _(E):
                for t in streams["act"]:
                    t(E)

            @block.vector
            def _(E):
                for t in streams["dve"]:
                    t(E)

            @block.gpsimd
            def _(E):
                for t in streams["pool"]:
                    t(E)

            @block.sync
            def _(E):
                for t in streams["sp"]:
                    t(E)
        for cm in reversed(self._ctx):
            cm.__exit__(None, None, None)


class Arena:
    def __init__(self, nc, es, words):
        self.t = es.enter_context(nc.sbuf_tensor("arena", [128, words], F32))
        self.top = 0
        self.words = words

    def f32(self, n):
        n8 = (n + 7) // 8 * 8
        off = self.top
        self.top += n8
        assert self.top <= self.words, ("arena overflow", self.top, self.words)
        return self.t[:, off:off + n]

    def bf16(self, n):
        w = (n + 1) // 2
        w8 = (w + 7) // 8 * 8
        off = self.top
        self.top += w8
        assert self.top <= self.words, ("arena overflow", self.top, self.words)
        return self.t[:, off:off + w].bitcast(BF16)


def build(stage=99):
    global _NEED
    _NEED = None
    _build(stage)
    _NEED = Sched.last_need
    nc = _build(stage)
    _NEED = None
    return nc


def _build(stage=99):
    nc = bass.Bass("TRN2", target_bir_lowering=False)
    es = contextlib.ExitStack()

    def din(name, shape, dt=F32):
        return nc.dram_tensor(name, list(shape), dt, kind="ExternalInput").ap()

    xT_d = din("xT", [128, KC, TH])
    amask_d = din("amask", [128, 2, 256])
    w_u_d = din("w_u", [8, 128, KC, 128])
    w_v_d = din("w_v", [128, KC, 1024])
    w_q_d = din("w_q", [8, 128, KC, 128])
    w_k_d = din("w_k", [128, KC, 128])
    w_vv_d = din("w_vv", [128, KC, 128])
    gvec_d = din("gvec", [128, 3, KC])
    wsT_d = din("wsT", [128, 8, 128])
    bs_bc_d = din("bs_bc", [128, 8, 128])
    sink_bc_d = din("sink_bc", [128, 16])
    ln_d = din("ln_gb", [128, 2, 8])
    w_o_d = din("w_o", [16, 128, KC, 128])
    w_qr_d = din("w_qr", [16, 128, KC, 128])
    keysT_d = din("keysT", [128, 2, 128])
    if stage >= 20:
        dwn_d = din("dwn", [128, 128, KC, 128])
        if stage not in (21, 22):
            upw_d = din("upw", [2, 128, 128, 1024])
    cst_d = din("cst", [128, 3, 128])
    yT_d = nc.dram_tensor("yT", [128, KC, T], F32, kind="ExternalOutput").ap()
    x1s_d = nc.dram_tensor("x1s", [128, KC, T], F32, kind="ExternalOutput").ap()
    h2s_d = nc.dram_tensor("h2s", [128, KC, 512], BF16, kind="ExternalOutput").ap()

    S = Sched(nc)
    A = Arena(nc, es, 53200)
    psA = es.enter_context(nc.psum_tensor("psA", [128, 4096], F32))
    pb = [psA[:, i * 512:(i + 1) * 512] for i in range(8)]
    PB = [f"pb{i}" for i in range(8)]

    def mm(out, lhsT, rhs, start, stop, reads, writes):
        S.op("pe", lambda E: E.matmul(out, lhsT=lhsT, rhs=rhs, start=start, stop=stop), reads, writes)

    def tr(out, in_, ident, reads, writes):
        S.op("pe", lambda E: E.transpose(out, in_, ident), reads, writes)

    def act(out, in_, func, reads, writes, bias=None, scale=None, accum_out=None, eng="act"):
        kw = {}
        if bias is not None:
            kw["bias"] = bias
        if scale is not None:
            kw["scale"] = scale
        if accum_out is not None:
            kw["accum_out"] = accum_out
        S.op("act", lambda E: E.activation(out=out, in_=in_, func=func, **kw), reads, writes)

    def tt(eng, out, in0, in1, op, reads, writes):
        S.op(eng, lambda E: E.tensor_tensor(out=out, in0=in0, in1=in1, op=op), reads, writes)

    def ts(eng, out, in0, s1, op0, reads, writes, s2=None, op1=None):
        if op1 is None:
            S.op(eng, lambda E: E.tensor_scalar(out=out, in0=in0, scalar1=s1, scalar2=None, op0=op0), reads, writes)
        else:
            S.op(eng, lambda E: E.tensor_scalar(out=out, in0=in0, scalar1=s1, scalar2=s2, op0=op0, op1=op1), reads, writes)

    def stt(eng, out, in0, scalar, in1, op0, op1, reads, writes):
        S.op(eng, lambda E: E.scalar_tensor_tensor(out=out, in0=in0, scalar=scalar, in1=in1, op0=op0, op1=op1), reads, writes)

    def cp(eng, out, in_, reads, writes):
        if eng == "act":
            S.op("act", lambda E: E.activation(out=out, in_=in_, func=AF.Copy), reads, writes)
        else:
            S.op(eng, lambda E: E.tensor_copy(out, in_), reads, writes)

    def red(eng, out, in_, op, reads, writes):
        S.op(eng, lambda E: E.tensor_reduce(out=out, in_=in_, axis=AX.X, op=op), reads, writes)

    def dma(q, sem, out, in_, reads, writes):
        S.dma(q, sem, lambda E: E.dma_start(out=out, in_=in_), reads, writes)


    def dbg_exit(src_fn, nchunks, keys):
        stg = [A.f32(1024), A.f32(1024)]
        for a in range(nchunks):
            cp("dve", stg[a % 2], src_fn(a), keys, [f"dbg{a % 2}"])
            dma("sp", "y", yT_d[:, a, :], stg[a % 2], [f"dbg{a % 2}"], ["y"])
        S.finish()
        es.close()
        return nc

    cst = A.f32(3 * 128).rearrange("p (a b) -> p a b", b=128)
    ident_f = cst[:, 0, :]
    iota_f = cst[:, 1, :]
    cmask = cst[:, 2, :]
    ident_b = A.bf16(128)
    ones_b = A.bf16(128)
    gvec = A.f32(3 * KC).rearrange("p (a b) -> p a b", b=KC)
    keysT = A.bf16(2 * 128).rearrange("p (a b) -> p a b", b=128)
    dma("sp", "c0", cst, cst_d, [], ["cst"])
    dma("sp", "c0", gvec, gvec_d, [], ["gvec"])
    dma("pool", "c1", keysT, keysT_d, [], ["keysT"])
    cp("dve", ident_b, ident_f, ["cst"], ["ident_b"])
    S.op("dve", lambda E: E.memset(ones_b, 1.0), [], ["ones_b"])
    mark0 = A.top

    hT = A.bf16(KC * TH).rearrange("p (a b) -> p a b", b=TH)
    catT = A.bf16(16 * T).rearrange("p (a b) -> p a b", b=T)
    amask = A.f32(512).rearrange("p (a b) -> p a b", b=256)
    sink_bc = A.f32(16)
    ln_gb = A.f32(16).rearrange("p (a b) -> p a b", b=8)
    wsb = A.bf16(1024).rearrange("p (a b) -> p a b", b=128)
    Cg = A.f32(1024).rearrange("p (a b) -> p a b", b=128)
    rstd = A.f32(TH)
    NWB = 3
    wbuf = [A.bf16(KC * 128).rearrange("p (a b) -> p a b", b=128) for _ in range(NWB)]
    xstg = [A.f32(512) for _ in range(2)]
    markX = A.top
    XW = KC * TH
    assert markX + XW <= A.words

    wsrcs = ([w_u_d[g] for g in range(8)] + [w_q_d[c] for c in range(8)] + [w_k_d, w_vv_d]
             + [w_o_d[dc] for dc in range(16)])
    wstate = {"issued": 0, "used": 0}

    def get_w(limit=None):
        lim = len(wsrcs) if limit is None else limit
        while wstate["issued"] < min(lim, wstate["used"] + NWB):
            n = wstate["issued"]
            i = n % NWB
            dma("pool", f"wb{i}", wbuf[i], wsrcs[n], [], [f"wbuf{i}"])
            wstate["issued"] += 1
        i = wstate["used"] % NWB
        wstate["used"] += 1
        return wbuf[i], f"wbuf{i}"

    rot = {}

    def nextpb(lo=5, hi=8):
        i = rot.get((lo, hi), lo)
        rot[(lo, hi)] = lo + (i + 1 - lo) % (hi - lo)
        return i

    dma("sp", "c2", amask, amask_d, [], ["amask"])
    dma("sp", "c2", sink_bc, sink_bc_d, [], ["sink_bc"])
    dma("sp", "c2", ln_gb, ln_d, [], ["ln_gb"])

    A.top = markX
    xT = A.f32(KC * TH).rearrange("p (a b) -> p a b", b=TH)
    for i in range(4):
        dma("sp" if i % 2 == 0 else "act", "x", xT[:, 4 * i:4 * i + 4, :], xT_d[:, 4 * i:4 * i + 4, :], [], [f"xT{k}" for k in range(4 * i, 4 * i + 4)])
    w_vh_hi = A.t[:, markX + XW:markX + XW + 4096].bitcast(BF16).rearrange("p (a b) -> p a b", b=512)
    for q4 in range(4):
        dma("pool", "wv", w_vh_hi[:, 4 * q4:4 * q4 + 4, :], w_v_d[:, 4 * q4:4 * q4 + 4, 0:512], [], ["w_vh0"])
    CB = [(0, 512), (512, 512), (1024, 128)]
    for kc in range(KC):
        act(hT[:, kc, :], xT[:, kc, :], AF.Square, [f"xT{kc}"], [f"hT{kc}"])
        for bi, (c0, cn) in enumerate(CB):
            mm(pb[bi][:, 0:cn], ones_b, hT[:, kc, c0:c0 + cn], kc == 0, kc == KC - 1, ["ones_b", f"hT{kc}"], [PB[bi]])
    for bi, (c0, cn) in enumerate(CB):
        ts("dve", rstd[:, c0:c0 + cn], pb[bi][:, 0:cn], 1.0 / D, ALU.mult, [PB[bi]], ["rstd"], s2=EPS, op1=ALU.add)
    act(rstd, rstd, AF.Sqrt, ["rstd"], ["rstd"])
    S.op("dve", lambda E: E.reciprocal(out=rstd, in_=rstd), ["rstd"], ["rstd"])
    for kc in range(KC):
        stt("dve", hT[:, kc, :], xT[:, kc, :], gvec[:, 0, kc:kc + 1], rstd, ALU.mult, ALU.mult,
            [f"xT{kc}", "gvec", "rstd"], [f"hT{kc}"])
    S.barrier()
    if stage == 11:
        A.top = markX + XW
        return dbg_exit(lambda a: hT[:, a, 128:TH], 16, [f"hT{k}" for k in range(KC)])

    A.top = markX
    uT = A.bf16(8 * T).rearrange("p (a b) -> p a b", b=T)
    vn_all = A.bf16(8 * 1024).rearrange("p (a g c) -> p a g c", g=8, c=128)
    w_vh = A.bf16(KC * 512).rearrange("p (a b) -> p a b", b=512)
    vg = [A.f32(512).rearrange("p (a b) -> p a b", b=128) for _ in range(2)]
    cen = [A.f32(512).rearrange("p (a b) -> p a b", b=128) for _ in range(2)]
    sqv = A.f32(512).rearrange("p (a b) -> p a b", b=128)
    st8 = [A.f32(4 * 4).rearrange("p (a b) -> p a b", b=4) for _ in range(2)]
    sgt = [A.f32(512).rearrange("p (a b) -> p a b", b=128) for _ in range(2)]
    wsf = A.f32(1024).rearrange("p (a b) -> p a b", b=128)
    bs_bc = A.f32(1024).rearrange("p (a b) -> p a b", b=128)
    assert A.top <= markX + XW, A.top - markX
    dma("sp", "c0", wsf, wsT_d, [], ["wsf"])
    dma("sp", "c0", bs_bc, bs_bc_d, [], ["bs_bc"])
    tt("dve", wsb, wsf, cmask[:, None, :].broadcast_to([128, 8, 128]), ALU.mult, ["wsf", "cst"], ["wsb"])
    for hb in range(2):
        mm(pb[3 + hb][:, :], ones_b, wsb[:, 4 * hb:4 * hb + 4, :].rearrange("p a b -> p (a b)"), True, True,
           ["ones_b", "wsb"], [PB[3 + hb]])
    for g in range(8):
        stt("dve", Cg[:, g, :], pb[3 + g // 4][:, (g % 4) * 128:(g % 4 + 1) * 128], ln_gb[:, 1, g:g + 1], bs_bc[:, g, :],
            ALU.mult, ALU.add, [PB[3 + g // 4], "ln_gb", "bs_bc"], ["Cg"])
    for q4 in range(4):
        dma("pool", "wv1", w_vh[:, 4 * q4:4 * q4 + 4, :], w_v_d[:, 4 * q4:4 * q4 + 4, 512:1024], [], ["w_vh1"])
    for hb in range(2):
        w_vb = w_vh_hi if hb == 0 else w_vh
        for tl in range(8):
            b = nextpb()
            i2 = tl % 2
            for kc in range(KC):
                mm(pb[b][:, :], hT[:, kc, 128 + tl * 128:128 + (tl + 1) * 128], w_vb[:, kc, :],
                   kc == 0, kc == KC - 1, [f"w_vh{hb}", f"hT{kc}"], [PB[b]])
            act(vg[i2].rearrange("p a b -> p (a b)"), pb[b][:, :], AF.Gelu, [PB[b]], [f"vg{i2}"])
            s8 = st8[i2]
            k8 = f"st8{i2}"
            red("dve", s8[:, 0, :], vg[i2], ALU.add, [f"vg{i2}"], [k8])
            ts("dve", s8[:, 1, :], s8[:, 0, :], -1.0 / 128, ALU.mult, [k8], [k8])
            tt("dve", cen[i2], vg[i2], s8[:, 1, :, None].broadcast_to([128, 4, 128]), ALU.add, [f"vg{i2}", k8], [f"cen{i2}"])
            tt("dve", sqv, cen[i2], cen[i2], ALU.mult, [f"cen{i2}"], ["sqv"])
            red("dve", s8[:, 2, :], sqv, ALU.add, ["sqv"], [k8])
            ts("dve", s8[:, 2, :], s8[:, 2, :], 1.0 / 128, ALU.mult, [k8], [k8], s2=EPS, op1=ALU.add)
            act(s8[:, 3, :], s8[:, 2, :], AF.Sqrt, [k8], [k8])
            S.op("dve", lambda E, s8=s8: E.reciprocal(out=s8[:, 3, :], in_=s8[:, 3, :]), [k8], [k8])
            tt("dve", vn_all[:, tl, 4 * hb:4 * hb + 4, :], cen[i2], s8[:, 3, :, None].broadcast_to([128, 4, 128]), ALU.mult,
               [f"cen{i2}", k8], [f"vn{tl}"])
    for g in range(8):
        wb, wk = get_w()
        for th in range(2):
            b = nextpb()
            for kc in range(KC):
                mm(pb[b][:, :], wb[:, kc, :], hT[:, kc, 128 + th * 512:128 + (th + 1) * 512], kc == 0, kc == KC - 1,
                   [wk, f"hT{kc}"], [PB[b]])
            act(uT[:, g, th * 512:(th + 1) * 512], pb[b][:, :], AF.Gelu, [PB[b]], ["uT"])
    for tl in range(8):
        for hb in range(2):
            b = nextpb()
            for g4 in range(4):
                g = hb * 4 + g4
                mm(pb[b][:, g4 * 128:(g4 + 1) * 128], vn_all[:, tl, g, :], wsb[:, g, :], True, True,
                   [f"vn{tl}", "wsb"], [PB[b]])
            sk = f"sgt{hb}"
            for g4 in range(4):
                g = hb * 4 + g4
                stt("dve", sgt[hb][:, g4, :], pb[b][:, g4 * 128:(g4 + 1) * 128], ln_gb[:, 0, g:g + 1], Cg[:, g, :],
                    ALU.mult, ALU.add, [PB[b], "ln_gb", "Cg"], [sk])
            tt("dve", catT[:, 4 * hb:4 * hb + 4, tl * 128:(tl + 1) * 128], sgt[hb], uT[:, 4 * hb:4 * hb + 4, tl * 128:(tl + 1) * 128],
               ALU.mult, [sk, "uT"], [f"cat{tl}"])
    S.barrier()
    if stage == 12:
        A.top = markX + XW
        return dbg_exit(lambda a: catT[:, a, :], 8, [f"cat{t}" for t in range(8)])

    A.top = markX
    qT = A.bf16(8 * T).rearrange("p (a b) -> p a b", b=T)
    kT = A.bf16(TH)
    vtok = A.bf16(9 * 128).rearrange("p (a b) -> p a b", b=128)
    kbd = A.bf16(8 * 512).rearrange("p (a b) -> p a b", b=512)
    sc = [A.f32(2048).rearrange("p (a b) -> p a b", b=256) for _ in range(3)]
    pn = [A.bf16(2048).rearrange("p (a b) -> p a b", b=256) for _ in range(2)]
    pT = [A.bf16(2048).rearrange("p (a b) -> p a b", b=128) for _ in range(2)]
    sm = [A.f32(64).rearrange("p (a b) -> p a b", b=8) for _ in range(3)]
    assert A.top <= markX + XW
    for c in range(8):
        wb, wk = get_w()
        for th in range(2):
            b = nextpb()
            for kc in range(KC):
                mm(pb[b][:, :], wb[:, kc, :], hT[:, kc, 128 + th * 512:128 + (th + 1) * 512], kc == 0, kc == KC - 1,
                   [wk, f"hT{kc}"], [PB[b]])
            ts("dve", qT[:, c, th * 512:(th + 1) * 512], pb[b][:, :], 0.125, ALU.mult, [PB[b]], ["qT"])
    wb, wk = get_w()
    for bi, (c0, cn) in enumerate(CB):
        b = nextpb()
        for kc in range(KC):
            mm(pb[b][:, 0:cn], wb[:, kc, :], hT[:, kc, c0:c0 + cn], kc == 0, kc == KC - 1, [wk, f"hT{kc}"], [PB[b]])
        cp("act", kT[:, c0:c0 + cn], pb[b][:, 0:cn], [PB[b]], ["kT"])
    wb, wk = get_w()
    for tl in range(9):
        b = nextpb()
        for kc in range(KC):
            mm(pb[b][:, 0:128], hT[:, kc, tl * 128:(tl + 1) * 128], wb[:, kc, :], kc == 0, kc == KC - 1,
               [wk, f"hT{kc}"], [PB[b]])
        cp("act", vtok[:, tl, :], pb[b][:, 0:128], [PB[b]], ["vtok"])
    S.op("dve", lambda E: E.memset(kbd, 0.0), [], ["kbd"])
    for blk in range(8):
        cp("dve", kbd[0:64, blk, 0:256], kT[0:64, blk * 128:blk * 128 + 256], ["kT"], ["kbd"])
        cp("act", kbd[64:128, blk, 256:512], kT[64:128, blk * 128:blk * 128 + 256], ["kT"], ["kbd"])
    if stage == 131:
        S.barrier()
        A.top = markX + XW
        return dbg_exit(lambda a: qT[:, a, :], 8, ["qT"])
    sink_v = sink_bc.rearrange("p (s c) -> p c s", s=2)
    iters = [(blk, c4) for blk in range(8) for c4 in range(2)]

    def att_s1a(n):
        blk, c4 = iters[n]
        i3 = n % 3
        mb = 0 if blk == 0 else 1
        for c_ in range(4):
            mm(pb[c_], qT[:, c4 * 4 + c_, blk * 128:(blk + 1) * 128], kbd[:, blk, :], True, True, ["qT", "kbd"], [PB[c_]])
        m_ = sm[i3]
        mk = f"sm{i3}"
        snk = sink_v[:, c4 * 4:c4 * 4 + 4, :]
        tt("dve", sc[i3], psA[:, 0:2048].rearrange("p (a b) -> p a b", b=256), amask[:, mb:mb + 1, :].broadcast_to([128, 8, 256]),
           ALU.add, PB[0:4] + ["amask"], [f"sc{i3}"])
        red("dve", m_[:, 0, :], sc[i3], ALU.max, [f"sc{i3}"], [mk])
        tt("dve", m_[:, 1, :].rearrange("p (c s) -> p c s", s=2), m_[:, 0, :].rearrange("p (c s) -> p c s", s=2), snk, ALU.max,
           [mk, "sink_bc"], [mk])
        ts("dve", m_[:, 2, :], m_[:, 1, :], -1.0, ALU.mult, [mk], [mk])
        tt("dve", m_[:, 4, :].rearrange("p (c s) -> p c s", s=2), snk, m_[:, 1, :].rearrange("p (c s) -> p c s", s=2), ALU.subtract,
           [mk, "sink_bc"], [mk])

    def att_s1b(n):
        blk, c4 = iters[n]
        i3 = n % 3
        i2 = n % 2
        m_ = sm[i3]
        mk = f"sm{i3}"
        for h in range(8):
            act(sc[i3][:, h, :], sc[i3][:, h, :], AF.Exp, [f"sc{i3}", mk], [f"sc{i3}", mk],
                bias=m_[:, 2, h:h + 1], scale=1.0, accum_out=m_[:, 3, h:h + 1])
        act(m_[:, 5, :], m_[:, 4, :], AF.Exp, [mk], [mk])
        tt("dve", m_[:, 6, :], m_[:, 5, :], m_[:, 3, :], ALU.add, [mk], [mk])
        S.op("dve", lambda E, m_=m_: E.reciprocal(out=m_[:, 7, :], in_=m_[:, 6, :]), [mk], [mk])
        tt("pool", pn[i2], sc[i3], m_[:, 7, :, None].broadcast_to([128, 8, 256]), ALU.mult, [f"sc{i3}", mk], [f"pn{i2}"])

    def att_s2(n):
        blk, c4 = iters[n]
        i2 = n % 2
        ptv = psA[:, 2048:3072].bitcast(BF16).rearrange("p (a b) -> p a b", b=128)
        for h in range(8):
            for kc2 in range(2):
                tr(ptv[:, h * 2 + kc2, :], pn[i2][:, h, kc2 * 128:(kc2 + 1) * 128], ident_b, [f"pn{i2}", "ident_b"], [PB[4 + h // 4]])
        cp("act", pT[i2], ptv, [PB[4], PB[5]], [f"pT{i2}"])
        for c_ in range(4):
            for s_ in range(2):
                lo, hi = s_ * 64, (s_ + 1) * 64
                for kc2 in range(2):
                    mm(pb[6][lo:hi, c_ * 128:(c_ + 1) * 128], vtok[:, blk + kc2, lo:hi], pT[i2][:, (c_ * 2 + s_) * 2 + kc2, :],
                       kc2 == 0, kc2 == 1, ["vtok", f"pT{i2}"], [PB[6]])
        cp("dve", catT[:, 8 + c4 * 4:8 + c4 * 4 + 4, blk * 128:(blk + 1) * 128], pb[6].rearrange("p (a b) -> p a b", b=128),
           [PB[6]], [f"cat{blk}"])

    NI = len(iters)
    att_s1a(0)
    att_s1a(1)
    att_s1b(0)
    for n in range(NI):
        if n + 2 < NI:
            att_s1a(n + 2)
        if n + 1 < NI:
            att_s1b(n + 1)
        att_s2(n)
    S.barrier()
    if stage == 13:
        A.top = markX + XW
        return dbg_exit(lambda a: catT[:, a, :], 16, [f"cat{t}" for t in range(8)])

    A.top = markX
    x1T = A.f32(KC * T).rearrange("p (a b) -> p a b", b=T)
    H2P_OFF = mark0 + 40256 + 5920 + 1024
    assert H2P_OFF >= markX + XW and H2P_OFF + 4096 <= A.words
    h2p0 = A.t[:, H2P_OFF:H2P_OFF + 4096].bitcast(BF16).rearrange("p (a b) -> p a b", b=512)
    CAT = [f"cat{t}" for t in range(8)]
    HT = [f"hT{k}" for k in range(KC)]

    def norm2_acc(kc):
        act(hT[:, kc, 0:T], x1T[:, kc, :], AF.Square, [f"x1T{kc}"], [f"hT{kc}"])
        for th in range(2):
            mm(pb[th][:, :], ones_b, hT[:, kc, th * 512:(th + 1) * 512], kc == 0, kc == KC - 1, ["ones_b", f"hT{kc}"], [PB[th]])
        dma("sp", "sp1", x1s_d[:, kc, :], x1T[:, kc, :], [f"x1T{kc}"], ["x1s"])
    xi = 0
    for dc in range(16):
        wb, wk = get_w()
        for th in range(2):
            b = nextpb()
            xs_ = xstg[xi % 2]
            xk = f"xstg{xi % 2}"
            xi += 1
            dma("sp", xk, xs_, xT_d[:, dc, 128 + th * 512:128 + (th + 1) * 512], [], [xk])
            for fc in range(KC):
                mm(pb[b][:, :], wb[:, fc, :], catT[:, fc, th * 512:(th + 1) * 512], fc == 0, fc == KC - 1,
                   [wk] + CAT[4 * th:4 * th + 4], [PB[b]])
            tt("dve", x1T[:, dc, th * 512:(th + 1) * 512], xs_, pb[b][:, :], ALU.add, [PB[b], xk], [f"x1T{dc}"])
        if dc > 0:
            norm2_acc(dc - 1)
    norm2_acc(KC - 1)
    for th in range(2):
        ts("dve", rstd[:, th * 512:(th + 1) * 512], pb[th][:, :], 1.0 / D, ALU.mult, [PB[th]], ["rstd"], s2=EPS, op1=ALU.add)
    act(rstd[:, 0:T], rstd[:, 0:T], AF.Sqrt, ["rstd"], ["rstd"])
    S.op("dve", lambda E: E.reciprocal(out=rstd[:, 0:T], in_=rstd[:, 0:T]), ["rstd"], ["rstd"])
    for kc in range(KC):
        stt("dve", h2p0[:, kc, :], x1T[:, kc, 0:512], gvec[:, 1, kc:kc + 1], rstd[:, 0:512], ALU.mult, ALU.mult,
            [f"x1T{kc}", "gvec", "rstd"], ["h2p"])
    for kc in range(KC):
        stt("dve", hT[:, kc, 512:T], x1T[:, kc, 512:T], gvec[:, 1, kc:kc + 1], rstd[:, 512:T], ALU.mult, ALU.mult,
            [f"x1T{kc}", "gvec", "rstd"], [f"hT{kc}"])
        if kc % 4 == 3:
            dma("sp", "sp2", h2s_d[:, kc - 3:kc + 1, :], hT[:, kc - 3:kc + 1, 512:T], HT[kc - 3:kc + 1], ["h2s"])
    S.barrier()

    TP = 512
    A.top = mark0
    Wb = A.bf16(128 * TP).rearrange("p (j t) -> p j t", t=TP)
    rank = A.f32(4 * TP).rearrange("p (a t) -> p a t", t=TP)
    q2T = A.bf16(8 * TP).rearrange("p (h t) -> p h t", t=TP)
    IfTb = A.bf16(TP)
    iota_b = A.bf16(128)
    cp("dve", iota_b, iota_f, ["cst"], ["iota_b"])
    wb2_off = A.top
    wbuf2 = [A.bf16(KC * 128).rearrange("p (a b) -> p a b", b=128) for _ in range(NWB)]
    WB2K = [f"wbuf2_{i}" for i in range(NWB)]
    markZ = A.top
    NWQ = NWB + 1
    WQ3_OFF = mark0 + 40256 + 5920 + 1024 + 4096
    assert WQ3_OFF + 1024 <= A.words
    wbuf2.append(A.t[:, WQ3_OFF:WQ3_OFF + 1024].bitcast(BF16).rearrange("p (a b) -> p a b", b=128))
    wq_srcs = [w_qr_d[cc] for cc in range(16)] * 2
    wq_state = {"issued": 0, "used": 0}

    def get_wq():
        lim = (wq_state["used"] // 16 + 1) * 16
        while wq_state["issued"] < min(lim, wq_state["used"] + NWQ):
            n = wq_state["issued"]
            i = n % NWQ
            dma("pool", f"wq{i}", wbuf2[i], wq_srcs[n], [], [f"wbuf2_{i}"])
            wq_state["issued"] += 1
        i = wq_state["used"] % NWQ
        wq_state["used"] += 1
        return wbuf2[i], f"wbuf2_{i}"

    x2T_e = A.t[:, mark0:mark0 + KC * T].rearrange("p (a b) -> p a b", b=T)
    for tp in range(2):
        tok0 = tp * TP
        A.top = markZ
        ND = 4
        dwb = [A.bf16(KC * 128).rearrange("p (a b) -> p a b", b=128) for _ in range(ND)]
        q1T = A.bf16(8 * TP).rearrange("p (h t) -> p h t", t=TP)
        cand = A.f32(256).rearrange("p (r c) -> p r c", c=16)
        sm8 = A.f32(32).rearrange("p (a h) -> p a h", h=8)
        rk = A.f32(512).rearrange("p (a h r) -> p a h r", h=8, r=16)
        assert A.top == H2P_OFF, (A.top, H2P_OFF)
        h2p = A.bf16(KC * TP).rearrange("p (a b) -> p a b", b=TP)
        scr = A.t[:, wb2_off:wb2_off + 3072]
        o_ = {"n": 0}

        def sf32(n):
            off = o_["n"]
            o_["n"] += (n + 7) // 8 * 8
            assert o_["n"] <= 3072
            return scr[:, off:off + n]
        s12 = sf32(2048).rearrange("p (a b) -> p a b", b=128)
        wk1 = sf32(128)
        wk2 = sf32(256)
        v1 = sf32(128).rearrange("p (h r) -> p h r", r=16)
        v2 = sf32(128).rearrange("p (h r) -> p h r", r=16)
        I1 = sf32(128).bitcast(U32).rearrange("p (h r) -> p h r", r=16)
        tv = sf32(128).rearrange("p (h r) -> p h r", r=16)
        ev = sf32(128).rearrange("p (h r) -> p h r", r=16)

        dst = {"issued": 0}

        def get_d(j):
            while dst["issued"] < min(128, j + ND):
                n = dst["issued"]
                dma("pool", f"dw{n % ND}", dwb[n % ND], dwn_d[n], [], [f"dwb{n % ND}"])
                dst["issued"] += 1
            return dwb[j % ND], f"dwb{j % ND}"

        for cc in range(16):
            wb, wk = get_wq()
            b = nextpb(4, 8)
            for kc in range(KC):
                mm(pb[b][:, :], wb[:, kc, :], h2p[:, kc, :], kc == 0, kc == KC - 1, [wk, "h2p"], [PB[b]])
            h, half = cc // 2, cc % 2
            if half == 0:
                cp("act", q1T[:, h, :], pb[b][:, :], [PB[b]], ["q1T"])
            else:
                cp("dve", q2T[:, h, :], pb[b][:, :], [PB[b]], ["q2T"])
            if cc == 11:
                get_d(0)

        def prepA(tl):
            tc0 = tl * 128
            for cc in range(16):
                h, half = cc // 2, cc % 2
                src = q1T if half == 0 else q2T
                mm(pb[4 + cc // 4][:, (cc % 4) * 128:(cc % 4 + 1) * 128], src[:, h, tc0:tc0 + 128], keysT[:, half, :], True, True,
                   ["q1T", "q2T", "keysT"], [PB[4 + cc // 4]])
            for b4 in range(4):
                cp("dve", s12[:, 4 * b4:4 * b4 + 4, :], pb[4 + b4][:, :].rearrange("p (a b) -> p a b", b=128), [PB[4 + b4]], ["s12"] + WB2K)
            for h in range(8):
                a1 = s12[:, 2 * h, :]
                a2 = s12[:, 2 * h + 1, :]
                S.op("dve", lambda E, a1=a1, h=h: E.max(out=v1[:, h, 0:8], in_=a1), ["s12"], ["v1"])
                S.op("dve", lambda E, a1=a1, h=h: E.max_index(out=I1[:, h, 0:8], in_max=v1[:, h, 0:8], in_values=a1), ["s12", "v1"], ["I1"])
                S.op("dve", lambda E, a1=a1, h=h: E.match_replace(out=wk1, in_to_replace=v1[:, h, 0:8], in_values=a1, imm_value=-1e30), ["s12", "v1"], ["wk1"])
                S.op("dve", lambda E, h=h: E.max(out=v1[:, h, 8:16], in_=wk1), ["wk1"], ["v1"])
                S.op("dve", lambda E, a1=a1, h=h: E.max_index(out=I1[:, h, 8:16], in_max=v1[:, h, 8:16], in_values=a1), ["s12", "v1"], ["I1"])
                S.op("dve", lambda E, a2=a2, h=h: E.max(out=v2[:, h, 0:8], in_=a2), ["s12"], ["v2"])
                S.op("dve", lambda E, a2=a2, h=h: E.match_replace(out=wk1, in_to_replace=v2[:, h, 0:8], in_values=a2, imm_value=-1e30), ["s12", "v2"], ["wk1"])
                S.op("dve", lambda E, h=h: E.max(out=v2[:, h, 8:16], in_=wk1), ["wk1"], ["v2"])
            ch = cand.rearrange("p r c -> p (r c)")
            for h in range(8):
                tt("dve", cand, v1[:, h, :, None].broadcast_to([128, 16, 16]), v2[:, h, None, :].broadcast_to([128, 16, 16]), ALU.add,
                   ["v1", "v2"], ["cand"])
                S.op("dve", lambda E, h=h: E.max(out=tv[:, h, 0:8], in_=ch), ["cand"], ["tv"])
                S.op("dve", lambda E, h=h: E.match_replace(out=wk2, in_to_replace=tv[:, h, 0:8], in_values=ch, imm_value=-1e30), ["cand", "tv"], ["wk2"])
                S.op("dve", lambda E, h=h: E.max(out=tv[:, h, 8:16], in_=wk2), ["wk2"], ["tv"])
            tt("dve", ev, tv, tv[:, :, 0:1].broadcast_to([128, 8, 16]), ALU.subtract, ["tv"], ["ev"])

        def prepB(tl):
            act(ev, ev, AF.Exp, ["ev"], ["ev"])
            red("dve", sm8[:, 0, :], ev, ALU.add, ["ev"], ["sm8"])
            ts("dve", sm8[:, 2, :], tv[:, :, 15], -1e-4, ALU.add, ["tv"], ["sm8"])
            tt("dve", rk[:, 0, :, :], sm8[:, 2, :, None].broadcast_to([128, 8, 16]), v1, ALU.subtract, ["sm8", "v1"], ["rk"])
            act(sm8[:, 3, :], sm8[:, 0, :], AF.Ln, ["sm8"], ["sm8"])
            tt("dve", sm8[:, 3, :], sm8[:, 3, :], v2[:, :, 0], ALU.add, ["sm8", "v2"], ["sm8"])
            tt("dve", sm8[:, 3, :], sm8[:, 3, :], v1[:, :, 0], ALU.add, ["sm8", "v1"], ["sm8"])
            tt("dve", rk[:, 3, :, :], v1, sm8[:, 3, :, None].broadcast_to([128, 8, 16]), ALU.subtract, ["v1", "sm8"], ["rk"])
            cp("dve", rk[:, 2, :, :], I1, ["I1"], ["rk"])
            cp("dve", rk[:, 1, :, :], I1, ["I1"], ["rk"])

        def prepC(tl):
            tc0 = tl * 128
            bT = 4 + tl % 2
            for a in range(4):
                tr(pb[bT][:, a * 128:(a + 1) * 128], rk[:, a, :, :].rearrange("p h r -> p (h r)"), ident_f, ["rk", "cst"], [PB[bT]])
            cp("dve", rank[:, :, tc0:tc0 + 128], pb[bT][:, :].rearrange("p (a b) -> p a b", b=128), [PB[bT]], ["rank"])
            cp("dve", IfTb[:, tc0:tc0 + 128], pb[bT][:, 256:384], [PB[bT]], ["rank"])

        hooks = {}
        for tl in range(4):
            j0 = 2 + 31 * tl
            hooks[j0] = (prepA, tl)
            hooks[j0 + 16] = (prepB, tl)
            hooks[j0 + 24] = (prepC, tl)

        for j in range(128):
            dw, dk = get_d(j)
            b = nextpb(0, 4)
            for kc in range(KC):
                mm(pb[b][:, :], dw[:, kc, :], h2p[:, kc, :], kc == 0, kc == KC - 1, [dk, "h2p"], [PB[b]])
            act(Wb[:, j, :], pb[b][:, :], AF.Gelu, [PB[b]], [f"W{j // 16}"])
            if j in hooks:
                fn_, tl_ = hooks[j]
                fn_(tl_)
        S.barrier()
        if stage == 21:
            S.finish()
            es.close()
            return nc

        A.top = markZ
        NB = 8
        q2rep = [A.bf16(NB * 128).rearrange("p (t m) -> p t m", m=128) for _ in range(2)]
        Lb = [A.bf16(NB * 128).rearrange("p (t m) -> p t m", m=128) for _ in range(2)]
        Rb = [A.bf16(NB * 128).rearrange("p (t m) -> p t m", m=128) for _ in range(2)]
        ebb = [A.bf16(NB * 128).rearrange("p (t m) -> p t m", m=128) for _ in range(2)]
        mskb = [A.bf16(NB * 128).rearrange("p (t m) -> p t m", m=128) for _ in range(2)]
        WK = [f"W{k}" for k in range(8)]
        nbat = TP // NB

        def pAv(i2):
            return psA[:, i2 * 1024:(i2 + 1) * 1024].rearrange("p (t m) -> p t m", m=128), [PB[2 * i2], PB[2 * i2 + 1]]

        def pGv(i2):
            return psA[:, 2048 + i2 * 1024:2048 + (i2 + 1) * 1024].rearrange("p (t m) -> p t m", m=128), [PB[4 + 2 * i2], PB[5 + 2 * i2]]

        def frontA1(tb):
            t0 = tb * NB
            i2 = tb % 2
            pA, kA = pAv(i2)
            cp("act", q2rep[i2].rearrange("p t (h r) -> p t h r", r=16),
               q2T[:, :, t0:t0 + NB].rearrange("p h t -> p t h")[:, :, :, None].broadcast_to([128, NB, 8, 16]), ["q2T"], [f"q2rep{i2}"])

        def frontA1mm(tb):
            i2 = tb % 2
            pA, kA = pAv(i2)
            for k in range(NB):
                mm(pA[:, k, :], q2rep[i2][:, k, :], keysT[:, 1, :], True, True, [f"q2rep{i2}", "keysT"], [kA[k // 4]])

        def frontA2(tb):
            t0 = tb * NB
            i2 = tb % 2
            pA, kA = pAv(i2)
            H = NB // 2

            def exps(hf):
                for k in range(hf * H, (hf + 1) * H):
                    act(ebb[i2][:, k, :], pA[:, k, :], AF.Exp, ["rank"], [f"eb{i2}{hf}", kA[hf]], bias=rank[:, 3, t0 + k:t0 + k + 1], scale=1.0)

            def mask(hf):
                tt("dve", mskb[i2][:, hf * H:(hf + 1) * H, :], pA[:, hf * H:(hf + 1) * H, :],
                   rank[:, 0, t0 + hf * H:t0 + (hf + 1) * H, None].broadcast_to([128, H, 128]), ALU.is_ge, ["rank"], [f"msk{i2}{hf}", kA[hf]])
            mask(0)
            exps(1)
            tt("dve", Lb[i2], iota_b[:, None, :].broadcast_to([128, NB, 128]), IfTb[:, t0:t0 + NB, None].broadcast_to([128, NB, 128]),
               ALU.is_equal, ["iota_b", "rank"], [f"L{i2}"])
            mask(1)
            exps(0)
            for hf in (1, 0):
                tt("pool", Rb[i2][:, hf * H:(hf + 1) * H, :], mskb[i2][:, hf * H:(hf + 1) * H, :], ebb[i2][:, hf * H:(hf + 1) * H, :], ALU.mult,
                   [f"msk{i2}{hf}", f"eb{i2}{hf}"], [f"R{i2}{hf}"])

        def backGmm(tb):
            i2 = tb % 2
            pG, kG = pGv(i2)
            H = NB // 2
            for k in list(range(H, NB)) + list(range(0, H)):
                mm(pG[:, k, :], Lb[i2][:, k, :], Rb[i2][:, k, :], True, True, [f"L{i2}", f"R{i2}{k // H}"], [kG[k // 4]])

        def backW(tb):
            t0 = tb * NB
            i2 = tb % 2
            pG, kG = pGv(i2)
            wv = Wb[:, :, t0:t0 + NB]
            tt("dve", wv, pG.rearrange("p t j -> p j t"), wv, ALU.mult, kG + WK, WK)

        frontA1(0)
        frontA1mm(0)
        if nbat > 1:
            frontA1(1)
            frontA1mm(1)
        frontA2(0)
        for tb in range(nbat):
            if tb + 2 < nbat:
                frontA1(tb + 2)
            backGmm(tb)
            if tb + 2 < nbat:
                frontA1mm(tb + 2)
            if tb + 1 < nbat:
                frontA2(tb + 1)
            backW(tb)
        S.barrier()
        if stage == 22:
            S.finish()
            es.close()
            return nc

        A.top = wb2_off
        NUB = 5
        xs3 = [A.f32(512) for _ in range(8)]
        upb = [A.bf16(2 * 1024).rearrange("p (a b) -> p a b", b=1024) for _ in range(NUB)]
        assert A.top <= H2P_OFF
        if tp == 0:
            dma("sp", "h2p", h2p, h2s_d, ["h2s"], ["h2p"])
        for dh in range(2):
            ust = {"issued": 0}

            def get_u(jp, dh=dh, ust=ust):
                while ust["issued"] < min(64, jp + NUB):
                    n = ust["issued"]
                    dma("pool", f"up{n % NUB}", upb[n % NUB], upw_d[dh, 2 * n:2 * n + 2].rearrange("a i n -> i a n"), [], [f"upb{n % NUB}"])
                    ust["issued"] += 1
                return upb[jp % NUB], f"upb{jp % NUB}"
            for dc in range(8):
                dma("sp", "xs3l", xs3[dc], x1s_d[:, dh * 8 + dc, tok0:tok0 + TP], ["x1s"], [f"xs3{dc}"])
            last = (tp == 1 and dh == 1)
            for j in range(128):
                ub, uk = get_u(j // 2)
                for dc in range(8):
                    mm(pb[dc], ub[:, j % 2, dc * 128:(dc + 1) * 128], Wb[:, j, :], j == 0, j == 127, [uk, f"W{j // 16}"], [PB[dc]])
                if last and j in (64, 80, 96, 108):
                    WD = ["W0", "W1", "W2", "W3"]
                    i = (64, 80, 96, 108).index(j)
                    tsl = slice(0, T) if i < 2 else slice(0, TP)
                    dma("act", "x2l", x2T_e[:, 4 * i:4 * i + 4, tsl], x1s_d[:, 4 * i:4 * i + 4, tsl], ["x2s", "x1s"],
                        [f"x2T{k}" for k in range(4 * i, 4 * i + 4)] + WD)
            for dc in range(8):
                kc = dh * 8 + dc
                if last:
                    tt("dve", x2T_e[:, kc, tok0:tok0 + TP], xs3[dc], pb[dc], ALU.add, [PB[dc], f"xs3{dc}"], [f"x2T{kc}"])
                else:
                    tt("dve", xs3[dc], xs3[dc], pb[dc], ALU.add, [PB[dc], f"xs3{dc}"], [f"xs3{dc}"])
                    dma("sp", "xo3", x1s_d[:, kc, tok0:tok0 + TP], xs3[dc], [f"xs3{dc}"], ["x2s"])
        S.barrier()

    A.top = mark0
    x2T = A.f32(KC * T).rearrange("p (a b) -> p a b", b=T)
    sq2 = A.bf16(KC * T).rearrange("p (a b) -> p a b", b=T)
    rs3 = A.f32(T)
    assert A.top - KC * T - KC * T // 2 - T == mark0
    for kc in range(KC):
        if kc % 3 == 2:
            tt("dve", sq2[:, kc, :], x2T[:, kc, :], x2T[:, kc, :], ALU.mult, [f"x2T{kc}"], [f"sq2{kc}"])
        else:
            act(sq2[:, kc, :], x2T[:, kc, :], AF.Square, [f"x2T{kc}"], [f"sq2{kc}"])
        for th in range(2):
            mm(pb[th][:, :], ones_b, sq2[:, kc, th * 512:(th + 1) * 512], kc == 0, kc == KC - 1, ["ones_b", f"sq2{kc}"], [PB[th]])
    for th in range(2):
        ts("dve", rs3[:, th * 512:(th + 1) * 512], pb[th][:, :], 1.0 / D, ALU.mult, [PB[th]], ["rs3"], s2=EPS, op1=ALU.add)
    act(rs3, rs3, AF.Sqrt, ["rs3"], ["rs3"])
    S.op("dve", lambda E: E.reciprocal(out=rs3, in_=rs3), ["rs3"], ["rs3"])
    for kc in range(KC):
        stt("dve", x2T[:, kc, :], x2T[:, kc, :], gvec[:, 2, kc:kc + 1], rs3, ALU.mult, ALU.mult, [f"x2T{kc}", "gvec", "rs3"], [f"x2T{kc}"])
    for i in range(4):
        dma("sp" if i % 2 == 0 else "act", "y", yT_d[:, 4 * i:4 * i + 4, :], x2T[:, 4 * i:4 * i + 4, :], [f"x2T{k}" for k in range(4 * i, 4 * i + 4)], ["y"])

    S.finish()
    es.close()
    return nc


def _prep(inputs):
    f = np.float32
    x = np.asarray(inputs["x"], f)
    w_in = np.asarray(inputs["w_in"], f)[0]
    common = {}

    def chunked(w, ncol_chunks):
        return np.ascontiguousarray(w.reshape(KC, 128, ncol_chunks, 128).transpose(2, 1, 0, 3))

    common["w_u"] = chunked(w_in[:, 0:1024], 8)
    common["w_v"] = np.ascontiguousarray(w_in[:, 1024:2048].reshape(KC, 128, 1024).transpose(1, 0, 2))
    wq = w_in[:, 2048:3072].reshape(D, 16, 64)
    wq_perm = np.stack([wq[:, 0:8], wq[:, 8:16]], axis=2).reshape(D, 1024)
    common["w_q"] = chunked(wq_perm, 8)
    common["w_k"] = np.ascontiguousarray(w_in[:, 3072:3200].reshape(KC, 128, 128).transpose(1, 0, 2))
    common["w_vv"] = np.ascontiguousarray(w_in[:, 3200:3328].reshape(KC, 128, 128).transpose(1, 0, 2))
    gv = np.stack([np.asarray(inputs["norm1_g"], f)[0], np.asarray(inputs["norm2_g"], f)[0], np.asarray(inputs["norm_f_g"], f)], 0)
    common["gvec"] = np.ascontiguousarray(gv.reshape(3, KC, 128).transpose(2, 0, 1))
    common["wsT"] = np.ascontiguousarray(np.asarray(inputs["w_spatial"], f)[0].transpose(2, 0, 1))
    common["bs_bc"] = np.ascontiguousarray(np.broadcast_to(np.asarray(inputs["b_spatial"], f)[0][None], (128, 8, 128)))
    common["sink_bc"] = np.ascontiguousarray(np.broadcast_to(np.asarray(inputs["attn_sinks"], f)[0][None], (128, 16)))
    common["ln_gb"] = np.ascontiguousarray(np.stack([np.asarray(inputs["sgu_ln_g"], f)[0].T, np.asarray(inputs["sgu_ln_b"], f)[0].T], 1))
    w_out = np.asarray(inputs["w_out"], f)[0]
    wo_a = w_out[0:1024]
    wo_b = w_out[1024:2048].reshape(16, 64, D)
    wo_bp = np.stack([wo_b[0:8], wo_b[8:16]], axis=1).reshape(1024, D)
    common["w_o"] = chunked(np.concatenate([wo_a, wo_bp], 0), 16)
    common["w_qr"] = chunked(np.asarray(inputs["w_query"], f)[0], 16)
    common["keysT"] = np.ascontiguousarray(np.asarray(inputs["sub_keys"], f)[0].transpose(2, 0, 1))
    ed = np.asarray(inputs["expert_down"], f)[0]
    common["dwn"] = np.ascontiguousarray(ed.reshape(128, 128, KC, 128).transpose(1, 3, 2, 0))
    eu = np.asarray(inputs["expert_up"], f)[0]
    common["upw"] = np.ascontiguousarray(eu.reshape(128, 128, 2, 1024).transpose(2, 1, 0, 3))
    ident = np.eye(128, dtype=f)
    iota = np.broadcast_to(np.arange(128, dtype=f)[None], (128, 128))
    cm = (np.arange(128)[:, None] <= np.arange(128)[None, :]).astype(f)
    common["cst"] = np.ascontiguousarray(np.stack([ident, iota, cm], 1))
    i = np.arange(128)[:, None]
    j = np.arange(256)[None, :]
    band = (j >= i + 1) & (j <= i + 128)
    m_all = np.where(band, 0.0, NEG).astype(f)
    m_first = np.where(band & (j >= 128), 0.0, NEG).astype(f)
    in_maps = []
    for c in range(NCORES):
        b, half = c // 2, c % 2
        t0 = half * T
        xs = np.zeros((TH, D), f)
        xs[128:] = x[b, t0:t0 + T]
        if half == 1:
            xs[:128] = x[b, t0 - 128:t0]
        m = dict(common)
        m["xT"] = np.ascontiguousarray(xs.T.reshape(KC, 128, TH).transpose(1, 0, 2))
        m["amask"] = np.ascontiguousarray(np.stack([m_first if half == 0 else m_all, m_all], 1))
        in_maps.append(m)
    return in_maps


def _gather(res):
    y = np.empty((4, 2048, D), np.float32)
    for c in range(NCORES):
        b, half = c // 2, c % 2
        yT = np.asarray(res.results[c]["yT"])
        y[b, half * T:(half + 1) * T] = yT.transpose(2, 1, 0).reshape(T, D)
    return y


def kernel(**inputs):
    in_maps = _prep(inputs)
    nc = build()
    res = run_bass_kernel_spmd(nc, in_maps, core_ids=list(range(NCORES)))
    return _gather(res)
```
